# Optimizing a Trainium2 kernel written in Bass

```python
import jax, jax.numpy as jnp
from jax import lax
import numpy as np

D_MODEL = 2048
BATCH = 2
SEQ = 8192
DEPTH = 1

MEM_LEN = 256
POOL_WINDOWS = (2, 4, 8, 16)
POOL_GROUP_DIM = D_MODEL // 16
POOL_WIDTH = len(POOL_WINDOWS) * POOL_GROUP_DIM
LRU_WIDTH = D_MODEL // 2
LRU_BLOCKS = 8
LRU_BLOCK_DIM = LRU_WIDTH // LRU_BLOCKS
CONV_WIDTH = 4
LRU_C = 8.0
MEM_HEADS = 4
MEM_HEAD_DIM = D_MODEL // 16
MEM_WIDTH = MEM_HEADS * MEM_HEAD_DIM
MIX_WIDTH = POOL_WIDTH + LRU_WIDTH + MEM_WIDTH
IN_WIDTH = POOL_WIDTH + 2 * LRU_WIDTH + MEM_WIDTH
N_GROUPS = 4
EXPERTS_PER_GROUP = 8
N_EXPERTS = N_GROUPS * EXPERTS_PER_GROUP
TOP_K = 2
EXPERT_FF = D_MODEL // 2
EXPERT_BLOCK = 128
ALPHA = (2 * DEPTH) ** 0.25
BETA = (8 * DEPTH) ** -0.25
EPS = 1e-5

kernel_name = 'hybrid_pool_rglru_memattn_hmoe_deepnorm'


def layer_norm(x, g, b):
    xf = x.astype(jnp.float32)
    mu = jnp.mean(xf, -1, keepdims=True)
    var = jnp.mean(jnp.square(xf - mu), -1, keepdims=True)
    return ((xf - mu) * lax.rsqrt(var + EPS) * g + b).astype(x.dtype)


def rms_norm(x, g):
    xf = x.astype(jnp.float32)
    return (xf * lax.rsqrt(jnp.mean(jnp.square(xf), -1, keepdims=True) + EPS) * g).astype(x.dtype)


def pool_mixer(u, w_pool, pool_scale):
    B, S, _ = u.shape
    uf = u.astype(jnp.float32)
    cs = jnp.cumsum(uf, axis=1)
    pos = jnp.arange(1, S + 1, dtype=jnp.float32)[None, :, None]
    parts = []
    for g, w in enumerate(POOL_WINDOWS):
        sl = slice(g * POOL_GROUP_DIM, (g + 1) * POOL_GROUP_DIM)
        c = cs[..., sl]
        prev = jnp.pad(c[:, :-w], ((0, 0), (w, 0), (0, 0)))
        parts.append((c - prev) / jnp.minimum(pos, float(w)) - uf[..., sl])
    d = jnp.stack(parts, axis=2).astype(u.dtype)
    y = jnp.einsum('bsgc,gcd->bsgd', d, w_pool).reshape(B, S, POOL_WIDTH)
    return y * pool_scale


def _linear_scan_combine(left, right):
    a_l, b_l = left
    a_r, b_r = right
    return a_l * a_r, a_r * b_l + b_r


def rglru_mixer(u, gate, conv_w, conv_b, w_a, b_a, w_x, b_x, lam):
    B, S, C = u.shape
    uc = lax.conv_general_dilated(u, conv_w[:, None, :], window_strides=(1,), padding=[(CONV_WIDTH - 1, 0)],
                                  dimension_numbers=('NWC', 'WIO', 'NWC'), feature_group_count=C) + conv_b
    ub = uc.reshape(B, S, LRU_BLOCKS, LRU_BLOCK_DIM)
    r = jax.nn.sigmoid(jnp.einsum('bshc,hcd->bshd', ub, w_a).reshape(B, S, C) + b_a).astype(jnp.float32)
    i = jax.nn.sigmoid(jnp.einsum('bshc,hcd->bshd', ub, w_x).reshape(B, S, C) + b_x).astype(jnp.float32)
    log_a = -LRU_C * r * jax.nn.softplus(-lam.astype(jnp.float32))
    a = jnp.exp(log_a)
    mult = jnp.sqrt(jnp.maximum(-jnp.expm1(2.0 * log_a), 0.0))
    bterm = mult * i * uc.astype(jnp.float32)
    _, h = lax.associative_scan(_linear_scan_combine, (a, bterm), axis=1)
    return jax.nn.gelu(gate) * h.astype(gate.dtype)


def memory_attention(q, mem, w_mem_kv):
    B, S, _ = q.shape
    M = mem.shape[1]
    kv = jnp.einsum('bmd,de->bme', mem, w_mem_kv)
    k = kv[..., :MEM_WIDTH].reshape(B, M, MEM_HEADS, MEM_HEAD_DIM)
    v = kv[..., MEM_WIDTH:].reshape(B, M, MEM_HEADS, MEM_HEAD_DIM)
    qh = q.reshape(B, S, MEM_HEADS, MEM_HEAD_DIM)
    s = jnp.einsum('bshd,bmhd->bhsm', qh, k).astype(jnp.float32) * (MEM_HEAD_DIM ** -0.5)
    p = jax.nn.softmax(s, axis=-1).astype(v.dtype)
    return jnp.einsum('bhsm,bmhd->bshd', p, v).reshape(B, S, MEM_WIDTH)


def mixer_sublayer(x, mem, w_in, w_pool, pool_scale, conv_w, conv_b, w_a, b_a, w_x, b_x, lam,
                   w_mem_kv, mix_norm_g, w_out):
    h = jnp.einsum('bsd,de->bse', x, w_in)
    o1 = POOL_WIDTH
    o2 = o1 + LRU_WIDTH
    o3 = o2 + LRU_WIDTH
    y_pool = pool_mixer(h[..., :o1], w_pool, pool_scale)
    y_lru = rglru_mixer(h[..., o1:o2], h[..., o2:o3], conv_w, conv_b, w_a, b_a, w_x, b_x, lam)
    y_mem = memory_attention(h[..., o3:], mem, w_mem_kv)
    m1 = POOL_WIDTH
    m2 = m1 + LRU_WIDTH
    mix = jnp.concatenate([rms_norm(y_pool, mix_norm_g[:m1]),
                           rms_norm(y_lru, mix_norm_g[m1:m2]),
                           rms_norm(y_mem, mix_norm_g[m2:])], axis=-1)
    return jnp.einsum('bse,ed->bsd', mix, w_out)


def hier_moe(x, w_group, b_group, w_fine, b_fine, w_gate, w_up, w_down):
    B, S, D = x.shape
    N = B * S
    xt = x.reshape(N, D)
    g_prob = jax.nn.softmax(jnp.dot(xt, w_group).astype(jnp.float32) + b_group, axis=-1)
    g_p, g_idx = lax.top_k(g_prob, 1)
    f_logits = jnp.dot(xt, w_fine).astype(jnp.float32).reshape(N, N_GROUPS, EXPERTS_PER_GROUP) + b_fine
    f_sel = jnp.take_along_axis(f_logits, g_idx[:, :, None], axis=1)[:, 0]
    top_l, top_j = lax.top_k(f_sel, TOP_K)
    top_w = jax.nn.softmax(top_l, axis=-1) * g_p
    e_idx = g_idx * EXPERTS_PER_GROUP + top_j
    A = N * TOP_K
    e_flat = e_idx.reshape(A)
    w_flat = top_w.reshape(A)
    tok_flat = jnp.repeat(jnp.arange(N, dtype=jnp.int32), TOP_K)
    order = jnp.argsort(e_flat, stable=True)
    e_sorted = e_flat[order]
    counts = jnp.bincount(e_flat, length=N_EXPERTS)
    starts = jnp.cumsum(counts) - counts
    padded = (counts + EXPERT_BLOCK - 1) // EXPERT_BLOCK * EXPERT_BLOCK
    pad_ends = jnp.cumsum(padded)
    pad_starts = pad_ends - padded
    dest = pad_starts[e_sorted] + jnp.arange(A, dtype=jnp.int32) - starts[e_sorted]
    n_blocks = (A + N_EXPERTS * EXPERT_BLOCK + EXPERT_BLOCK - 1) // EXPERT_BLOCK
    P = n_blocks * EXPERT_BLOCK
    buf_tok = jnp.full((P,), N, jnp.int32).at[dest].set(tok_flat[order])
    buf_w = jnp.zeros((P,), jnp.float32).at[dest].set(w_flat[order])
    block_start = jnp.arange(n_blocks, dtype=jnp.int32) * EXPERT_BLOCK
    block_exp = jnp.minimum(jnp.searchsorted(pad_ends, block_start, side='right'), N_EXPERTS - 1)
    x_pad = jnp.concatenate([xt, jnp.zeros((1, D), xt.dtype)], axis=0)
    xb = x_pad[buf_tok].reshape(n_blocks, EXPERT_BLOCK, D)

    def expert_block(args):
        xblk, e = args
        hdn = jax.nn.silu(xblk @ w_gate[e]) * (xblk @ w_up[e])
        return hdn @ w_down[e]

    yb = lax.map(expert_block, (xb, block_exp)).reshape(P, D)
    yb = yb * buf_w[:, None].astype(yb.dtype)
    out = jnp.zeros((N + 1, D), yb.dtype).at[buf_tok].add(yb)[:N]
    return out.reshape(B, S, D)


def setup_inputs(seed: int = 0) -> dict:
    key = jax.random.key(seed)
    ks = jax.random.split(key, 28)
    nrm = lambda k, shape, scale: jax.random.normal(k, shape, jnp.float32) * scale
    L, D = DEPTH, D_MODEL
    u = jax.random.uniform(ks[10], (L, LRU_WIDTH), jnp.float32, minval=0.9, maxval=0.999)
    s = u ** (1.0 / LRU_C)
    lam = jnp.log(s) - jnp.log1p(-s)
    w_mem_kv = jnp.concatenate([nrm(ks[11], (L, D, MEM_WIDTH), D ** -0.5),
                                nrm(ks[12], (L, D, MEM_WIDTH), BETA * D ** -0.5)], axis=-1)
    return {
        'x': nrm(ks[0], (BATCH, SEQ, D), 1.0),
        'mem': nrm(ks[1], (BATCH, MEM_LEN, D), 1.0),
        'w_in': nrm(ks[2], (L, D, IN_WIDTH), D ** -0.5),
        'w_pool': nrm(ks[3], (L, len(POOL_WINDOWS), POOL_GROUP_DIM, POOL_GROUP_DIM), POOL_GROUP_DIM ** -0.5),
        'pool_scale': 1.0 + nrm(ks[4], (L, POOL_WIDTH), 0.1),
        'conv_w': nrm(ks[5], (L, CONV_WIDTH, LRU_WIDTH), CONV_WIDTH ** -0.5),
        'conv_b': nrm(ks[6], (L, LRU_WIDTH), 0.02),
        'w_a': nrm(ks[7], (L, LRU_BLOCKS, LRU_BLOCK_DIM, LRU_BLOCK_DIM), LRU_BLOCK_DIM ** -0.5),
        'b_a': nrm(ks[8], (L, LRU_WIDTH), 0.02),
        'w_x': nrm(ks[9], (L, LRU_BLOCKS, LRU_BLOCK_DIM, LRU_BLOCK_DIM), LRU_BLOCK_DIM ** -0.5),
        'b_x': nrm(ks[13], (L, LRU_WIDTH), 0.02),
        'lam': lam,
        'w_mem_kv': w_mem_kv,
        'mix_norm_g': 1.0 + nrm(ks[14], (L, MIX_WIDTH), 0.02),
        'w_out': nrm(ks[15], (L, MIX_WIDTH, D), BETA * MIX_WIDTH ** -0.5),
        'ln1_g': 1.0 + nrm(ks[16], (L, D), 0.02),
        'ln1_b': nrm(ks[17], (L, D), 0.02),
        'w_group': nrm(ks[18], (L, D, N_GROUPS), D ** -0.5),
        'b_group': nrm(ks[19], (L, N_GROUPS), 0.01),
        'w_fine': nrm(ks[20], (L, D, N_EXPERTS), D ** -0.5),
        'b_fine': nrm(ks[21], (L, N_GROUPS, EXPERTS_PER_GROUP), 0.01),
        'w_gate': nrm(ks[22], (L, N_EXPERTS, D, EXPERT_FF), D ** -0.5),
        'w_up': nrm(ks[23], (L, N_EXPERTS, D, EXPERT_FF), D ** -0.5),
        'w_down': nrm(ks[24], (L, N_EXPERTS, EXPERT_FF, D), BETA * EXPERT_FF ** -0.5),
        'ln2_g': 1.0 + nrm(ks[25], (L, D), 0.02),
        'ln2_b': nrm(ks[26], (L, D), 0.02),
    }


def reference(x, mem, w_in, w_pool, pool_scale, conv_w, conv_b, w_a, b_a, w_x, b_x, lam, w_mem_kv,
              mix_norm_g, w_out, ln1_g, ln1_b, w_group, b_group, w_fine, b_fine, w_gate, w_up, w_down,
              ln2_g, ln2_b):
    for l in range(DEPTH):
        y = mixer_sublayer(x, mem, w_in[l], w_pool[l], pool_scale[l], conv_w[l], conv_b[l], w_a[l], b_a[l],
                           w_x[l], b_x[l], lam[l], w_mem_kv[l], mix_norm_g[l], w_out[l])
        x = layer_norm(ALPHA * x + y, ln1_g[l], ln1_b[l])
        y = hier_moe(x, w_group[l], b_group[l], w_fine[l], b_fine[l], w_gate[l], w_up[l], w_down[l])
        x = layer_norm(ALPHA * x + y, ln2_g[l], ln2_b[l])
    return x
```

```python
import numpy as np
from contextlib import ExitStack
import concourse.bass as bass
import concourse.mybir as mybir
from concourse.bass_utils import run_bass_kernel_spmd

F32 = mybir.dt.float32
F32R = mybir.dt.float32r
BF16 = mybir.dt.bfloat16
I32 = mybir.dt.int32
AF = mybir.ActivationFunctionType
ALU = mybir.AluOpType
AX = mybir.AxisListType

D = 2048
T = 2048
NCH = 16
NPRE = 12
CH = 512
CAP = 384
NSB = CAP // 128
NE = 32
ALPHA = 2.0 ** 0.25
EPS = 1e-5
DEBUG = False

O_PS, O_CW, O_CB, O_BA, O_BX, O_LAM, O_G, O_VM = 0, 4, 36, 44, 52, 60, 68, 84
NV = 100


class Prog:
    def __init__(self, nc, stack):
        self.nc = nc
        self.stack = stack
        self.eng = {"pe": nc.tensor, "act": nc.scalar, "dve": nc.vector,
                    "pool": nc.gpsimd, "sp": nc.sync}
        self.sem = {}
        for e in ("pe", "act", "dve", "pool"):
            self.sem[e] = stack.enter_context(nc.semaphore("sem_" + e))
        self.cnt = {e: 0 for e in ("pe", "act", "dve", "pool")}
        self.dsem = {}
        self.dcnt = {}
        self.last_w = {}
        self.readers = {}
        self.waited = {e: {} for e in ("pe", "act", "dve", "pool", "sp")}
        self.n_inst = 0

    def _tok_sem(self, tok):
        return self.sem[tok[1]] if tok[0] == "e" else self.dsem[tok[1]]

    def _wait(self, eng, tok):
        kind, key, val = tok
        name = (kind, key)
        if self.waited[eng].get(name, 0) >= val:
            return
        if kind == "e" and key == eng and eng == "pe":
            return
        self.waited[eng][name] = val
        self.eng[eng].wait_ge(self._tok_sem(tok), val)

    def _deps(self, eng, reads, writes):
        deps = []
        for r in reads:
            t = self.last_w.get(r)
            if t is not None:
                deps.append(t)
        for w in writes:
            t = self.last_w.get(w)
            if t is not None and not (t[0] == "e" and t[1] == eng):
                deps.append(t)
            for t in self.readers.get(w, ()):
                if not (t[0] == "e" and t[1] == eng):
                    deps.append(t)
        return deps

    def _commit(self, tok, reads, writes):
        for r in reads:
            self.readers.setdefault(r, []).append(tok)
        for w in writes:
            self.last_w[w] = tok
            self.readers[w] = []

    def op(self, eng, fn, reads=(), writes=(), inc=True):
        for t in self._deps(eng, reads, writes):
            self._wait(eng, t)
        ins = fn(self.eng[eng])
        self.n_inst += 1
        if inc:
            self.cnt[eng] += 1
            ins.then_inc(self.sem[eng], 1)
            tok = ("e", eng, self.cnt[eng])
        else:
            tok = ("e", eng, self.cnt[eng] + 1)
        self._commit(tok, reads, writes)
        return ins

    def dma(self, q, fn, reads=(), writes=(), key=None):
        if key is None:
            key = writes[0]
        if key not in self.dsem:
            self.dsem[key] = self.stack.enter_context(self.nc.semaphore("d_" + str(key)))
            self.dcnt[key] = 0
        for t in self._deps(q, reads, writes):
            self._wait(q, t)
        ins = fn(self.eng[q])
        self.n_inst += 1
        self.dcnt[key] += 16
        ins.then_inc(self.dsem[key], 16)
        tok = ("d", key, self.dcnt[key])
        self._commit(tok, reads, writes)
        return ins

    def _all_tokens(self):
        toks = [("e", e, self.cnt[e]) for e in ("pe", "act", "dve", "pool") if self.cnt[e] > 0]
        toks += [("d", k, v) for k, v in self.dcnt.items() if v > 0]
        return toks

    def barrier(self):
        toks = self._all_tokens()
        for e in ("pe", "act", "dve", "pool", "sp"):
            for t in toks:
                self._wait(e, t)
        self.last_w.clear()
        self.readers.clear()

    def finish(self, eng):
        for t in self._all_tokens():
            self._wait(eng, t)


def build_nc():
    nc = bass.Bass("TRN2", target_bir_lowering=False)
    nc.dge_precook = False
    dram = lambda name, shape, dt, kind="ExternalInput": nc.dram_tensor(name, shape, dt, kind=kind)
    xe = dram("xe", [NCH * CH, D], F32)
    mem = dram("mem", [256, D], F32)
    w_in_t = dram("w_in_t", [24, 128, 16 * 128], F32)
    w_out_t = dram("w_out_t", [128, 16, D], F32)
    w_kv_t = dram("w_kv_t", [128, 16, 1024], F32)
    w_pool_t = dram("w_pool_t", [128, 4 * 128], F32)
    w_a_t = dram("w_a_t", [128, 8 * 128], F32)
    w_x_t = dram("w_x_t", [128, 8 * 128], F32)
    chv_d = dram("chv", [128, NV], F32)
    invc_d = dram("invcnt", [128, 64], F32)
    lnp = dram("lnp", [4, D], F32)
    w_r_t = dram("w_r_t", [128, 16 * 36], F32)
    b_r = dram("b_r", [1, 36], F32)
    wg_t = dram("wg_t", [NE, 2, 128, 16 * 512], F32R)
    wu_t = dram("wu_t", [NE, 2, 128, 16 * 512], F32R)
    wd_t = dram("wd_t", [NE, 2, 128, 8 * 1024], F32R)
    out = dram("out", [T, D], F32, kind="ExternalOutput")
    Xs = dram("Xs", [NE * CAP, D], F32, kind="Internal")
    Ys = dram("Ys", [NE * CAP + 1, D], F32, kind="Internal")
    X1s = dram("X1s", [T, D], F32, kind="Internal")
    if DEBUG:
        dbg_x1 = dram("dbg_x1", [T, D], F32, kind="ExternalOutput")
        dbg_rt = dram("dbg_rt", [T, 4], F32, kind="ExternalOutput")

    with ExitStack() as g:
        P = Prog(nc, g)

        def sbin(st, name, shape, dt):
            return st.enter_context(nc.sbuf_tensor(name, shape, dt))

        pb = [g.enter_context(nc.psum_tensor("pb%d" % i, [128, 512], F32)) for i in range(8)]
        PB = ["pb%d" % i for i in range(8)]

        ident = sbin(g, "ident", [128, 128], F32)
        ones_f = sbin(g, "ones_f", [128, 128], F32)
        ones_b = sbin(g, "ones_b", [128, 128], BF16)
        ltri = sbin(g, "ltri", [128, 128], F32)
        chv = sbin(g, "chv_s", [128, NV], F32)
        invc = sbin(g, "invc_s", [128, 64], F32)
        cl = sbin(g, "cl", [128, 32], F32)
        vmh = sbin(g, "vmh", [128, 16], F32)
        wpool_b = sbin(g, "wpool_b", [128, 4 * 128], BF16)
        wa_b = sbin(g, "wa_b", [128, 8 * 128], BF16)
        wx_b = sbin(g, "wx_b", [128, 8 * 128], BF16)
        kT = sbin(g, "kT", [128, 4, 256], BF16)
        vv = sbin(g, "vv", [128, 2, 512], BF16)
        hst = sbin(g, "hst", [128, 8], F32)
        w_r = sbin(g, "w_r", [128, 16, 36], F32)
        b_rb = sbin(g, "b_rb", [128, 36], F32)
        rcb = sbin(g, "rcb", [128, 32], F32)
        sbase = sbin(g, "sbase", [128, 32], F32)
        dest_i = sbin(g, "dest_i", [128, 32], I32)
        w12 = sbin(g, "w12", [128, 32], F32)
        sm = sbin(g, "sm", [128, 64], F32)
        sm32 = sbin(g, "sm32", [128, 8, 36], F32)

        dma = P.dma
        op = P.op

        op("pool", lambda e: e.memset(ident[:], 0.0), writes=["ident"])
        op("pool", lambda e: e.affine_select(out=ident[:], in_=ident[:], pattern=[[-1, 128]],
                                             compare_op=ALU.not_equal, fill=1.0, base=0, channel_multiplier=1),
           reads=["ident"], writes=["ident"])
        op("pool", lambda e: e.memset(ones_f[:], 1.0), writes=["ones_f"])
        op("pool", lambda e: e.memset(ones_b[:], 1.0), writes=["ones_b"])
        op("pool", lambda e: e.affine_select(out=ltri[:], in_=ones_f[:], pattern=[[1, 128]],
                                             compare_op=ALU.is_gt, fill=0.0, base=0, channel_multiplier=-1),
           reads=["ones_f"], writes=["ltri"])
        op("pool", lambda e: e.memset(hst[:], 0.0), writes=["hst"])
        op("pool", lambda e: e.iota(dest_i[:], pattern=[[CAP, 32]], base=0, channel_multiplier=0), writes=["dest_i"])
        op("dve", lambda e: e.tensor_copy(out=sbase[:], in_=dest_i[:]), reads=["dest_i"], writes=["sbase"])
        op("pool", lambda e: e.memset(rcb[:], 0.0), writes=["rcb"])
        dma("sp", lambda e: e.dma_start(out=chv[:], in_=chv_d.ap()), writes=["chv"])
        dma("sp", lambda e: e.dma_start(out=invc[:], in_=invc_d.ap()), writes=["invc"])
        dma("sp", lambda e: e.dma_start(out=w_r[:], in_=w_r_t.ap().rearrange("p (k n) -> p k n", n=36)), writes=["w_r"])
        dma("sp", lambda e: e.dma_start(out=b_rb[:], in_=b_r.ap().partition_broadcast(128)), writes=["b_rb"])
        op("act", lambda e: e.activation(out=sm[:, 0:8], in_=chv[:, O_LAM:O_LAM + 8], func=AF.Exp, scale=-1.0),
           reads=["chv"], writes=["sm"])
        op("act", lambda e: e.activation(out=sm[:, 8:16], in_=sm[:, 0:8], func=AF.Ln, bias=1.0),
           reads=["sm"], writes=["sm"])
        op("dve", lambda e: e.tensor_scalar_mul(out=cl[:, 0:8], in0=sm[:, 8:16], scalar1=-4.0), reads=["sm"], writes=["cl"])
        op("dve", lambda e: e.tensor_scalar_mul(out=cl[:, 8:16], in0=sm[:, 8:16], scalar1=-8.0), reads=["sm"], writes=["cl"])
        op("dve", lambda e: e.tensor_scalar_mul(out=cl[:, 16:24], in0=chv[:, O_BA:O_BA + 8], scalar1=0.5), reads=["chv"], writes=["cl"])
        op("dve", lambda e: e.tensor_scalar_mul(out=cl[:, 24:32], in0=chv[:, O_BX:O_BX + 8], scalar1=0.5), reads=["chv"], writes=["cl"])
        op("dve", lambda e: e.tensor_scalar_mul(out=vmh[:], in0=chv[:, O_VM:O_VM + 16], scalar1=0.5), reads=["chv"], writes=["vmh"])

        with ExitStack() as sa:
            stg = sbin(sa, "stgA", [128, 1024], F32)
            memx = sbin(sa, "memx", [128, 2, D], F32)
            memT = sbin(sa, "memT", [128, 16, 256], BF16)
            wkv_s = [sbin(sa, "wkv_s%d" % i, [128, 1024], F32) for i in range(2)]
            wkv_b = [sbin(sa, "wkv_b%d" % i, [128, 1024], BF16) for i in range(2)]
            for (src, dstb, n, nm) in ((w_pool_t, wpool_b, 512, "wpool_b"), (w_a_t, wa_b, 1024, "wa_b"), (w_x_t, wx_b, 1024, "wx_b")):
                dma("sp", lambda e, src=src, n=n: e.dma_start(out=stg[:, 0:n], in_=src.ap()), writes=["stgA"])
                op("dve", lambda e, dstb=dstb, n=n: e.tensor_copy(out=dstb[:], in_=stg[:, 0:n]), reads=["stgA"], writes=[nm])
            for mt in range(2):
                dma("sp", lambda e, mt=mt: e.dma_start(out=memx[:, mt, :], in_=mem[mt * 128:(mt + 1) * 128, :]), writes=["memx%d" % mt])
            for mt in range(2):
                for g4 in range(4):
                    bk = g4 % 2
                    for i4 in range(4):
                        kc = g4 * 4 + i4
                        op("pe", lambda e, mt=mt, kc=kc, i4=i4, bk=bk: e.transpose(pb[bk][:, i4 * 128:(i4 + 1) * 128], memx[:, mt, kc * 128:(kc + 1) * 128], ident[:]),
                           reads=["memx%d" % mt, "ident"], writes=[PB[bk]], inc=(i4 == 3))
                    op("act" if g4 % 2 == 0 else "dve",
                       (lambda e, mt=mt, g4=g4, bk=bk: e.activation(out=memT[:, g4 * 4:(g4 + 1) * 4, mt * 128:(mt + 1) * 128], in_=pb[bk][:].rearrange("p (a b) -> p a b", b=128), func=AF.Copy))
                       if g4 % 2 == 0 else
                       (lambda e, mt=mt, g4=g4, bk=bk: e.tensor_copy(out=memT[:, g4 * 4:(g4 + 1) * 4, mt * 128:(mt + 1) * 128], in_=pb[bk][:].rearrange("p (a b) -> p a b", b=128))),
                       reads=[PB[bk]], writes=["memT"])
            for kc in range(16):
                s = kc % 2
                dma("sp", lambda e, kc=kc, s=s: e.dma_start(out=wkv_s[s][:], in_=w_kv_t[:, kc, :]), writes=["wkv_s%d" % s])
                op("dve", lambda e, s=s: e.tensor_copy(out=wkv_b[s][:], in_=wkv_s[s][:]), reads=["wkv_s%d" % s], writes=["wkv_b%d" % s])
                for h in range(4):
                    bk = h
                    op("pe", lambda e, h=h, kc=kc, s=s, bk=bk: e.matmul(pb[bk][:, 0:256], wkv_b[s][:, h * 128:(h + 1) * 128], memT[:, kc, :],
                                                                        start=(kc == 0), stop=(kc == 15)),
                       reads=["wkv_b%d" % s, "memT"], writes=[PB[bk]], inc=False)
                for mc in range(2):
                    op("pe", lambda e, mc=mc, kc=kc, s=s: e.matmul(pb[4 + mc][:], memT[:, kc, mc * 128:(mc + 1) * 128], wkv_b[s][:, 512:1024],
                                                                   start=(kc == 0), stop=(kc == 15)),
                       reads=["wkv_b%d" % s, "memT"], writes=[PB[4 + mc]], inc=(mc == 1))
            for h in range(4):
                bk = h
                op("act", lambda e, h=h, bk=bk: e.activation(out=kT[:, h, :], in_=pb[bk][:, 0:256], func=AF.Copy),
                   reads=[PB[bk]], writes=["kT"])
            for mc in range(2):
                op("dve", lambda e, mc=mc: e.tensor_copy(out=vv[:, mc, :], in_=pb[4 + mc][:]), reads=[PB[4 + mc]], writes=["vv"])
            P.barrier()

        with ExitStack() as sbs:
            yT = sbin(sbs, "yT", [128, 16, T], BF16)
            sp1 = sbs.enter_context(ExitStack())
            uext = sbin(sp1, "uext", [128, 8, CH + 3], BF16)
            upx = sbin(sp1, "upx", [128, 4, CH + 16], F32)
            op("pool", lambda e: e.memset(uext[:], 0.0), writes=["uext%d" % i for i in range(8)])
            op("pool", lambda e: e.memset(upx[:], 0.0), writes=["upx%d" % i for i in range(4)])
            yT_box = [yT]
            dg = sbin(sp1, "dg", [128, 8, 4, 128], BF16)
            for blk_ in range(8):
                for k_ in range(4):
                    cwc = O_CW + blk_ * 4 + k_
                    op("dve" if (blk_ * 4 + k_) % 2 == 0 else "pool",
                       lambda e, blk_=blk_, k_=k_, cwc=cwc: e.tensor_scalar(out=dg[:, blk_, k_, :], in0=ident[:], scalar1=chv[:, cwc:cwc + 1], scalar2=None, op0=ALU.mult),
                       reads=["ident", "chv"], writes=["dg"])

            def phase1(chunks, NS, own_phase):
              with ExitStack() as s1:
                yT = yT_box[0]
                tag = "o_" if own_phase else "p_"
                xin = [sbin(s1, tag + "xin%d" % i, [128, D], F32) for i in range(2)]
                xT = [sbin(s1, tag + "xT%d" % i, [128, 16, CH], BF16) for i in range(2)]
                wbf = [sbin(s1, tag + "wbf%d" % i, [128, 16, 128], BF16) for i in range(3)]
                NW = 9
                A = []
                for si in range(2):
                    tl = [sbin(s1, tag + "A%d_%d" % (si, i), [128, CH], F32) for i in range(4)]
                    A.append([tl[0][:], tl[1][:], tl[2][:], tl[3][:], tl[1][:]])
                ucbs = sbin(s1, tag + "ucbs", [128, 2, CH], BF16)
                ucb_aps = [ucbs[:, 0, :], ucbs[:, 1, :]]
                if NS > 2:
                    def ht(i):
                        return yT[:, i // 2, :].bitcast(F32)[:, (i % 2) * CH:(i % 2 + 1) * CH]
                    for si in range(2, NS):
                        tl = [ht(4 * (si - 2) + j_) for j_ in range(4)]
                        A.append([tl[0], tl[1], tl[2], tl[3], tl[1]])
                        ucb_aps.append(yT[:, 12 + (si - 2) // 4, ((si - 2) % 4) * CH:((si - 2) % 4 + 1) * CH])

                def akeys(st):
                    return ["A%d_0" % st, "A%d_1" % st, "A%d_2" % st, "A%d_3" % st, "A%d_1" % st]

                if own_phase:
                    W = [None] * 6 + [sbin(s1, tag + "W%d" % i, [128, CH + 16], F32) for i in range(6, NW)]
                    Wb = [None, sbin(s1, tag + "Wb1", [128, 2, CH], BF16), sbin(s1, tag + "Wb2", [128, 1, CH], BF16)]
                    vg = sbin(s1, tag + "vg", [128, 3, CH], F32)

                items = []
                for ci in chunks:
                    own = ci >= NPRE
                    for pr in range(4):
                        for blk in (2 * pr, 2 * pr + 1):
                            items.append((ci, 4 + blk, "u", blk))
                        if own:
                            items.append((ci, pr, "p", pr))
                            for blk in (2 * pr, 2 * pr + 1):
                                items.append((ci, 12 + blk, "g", blk))
                            items.append((ci, 20 + pr, "m", pr))
                    if ci == NPRE - 1:
                        for gi in range(4):
                            items.append((ci, gi, "ph", gi))
                NI = len(items)

                def load_w(n):
                    ci, j, kind, idx = items[n]
                    s = n % 3
                    dma("pool", lambda e: e.dma_start(out=wbf[s][:].rearrange("p a b -> p (a b)"), in_=w_in_t[j], max_dma_last_dim=4096),
                        writes=["wbf%d" % s])

                def prep_chunk(ci):
                    b = ci % 2
                    for tt in range(4):
                        xi = tt % 2
                        r0 = ci * CH + tt * 128
                        dma("sp", lambda e: e.dma_start(out=xin[xi][:], in_=xe[r0:r0 + 128, :]), writes=["xin%d" % xi])
                        for g4 in range(4):
                            bk = 0
                            for i4 in range(4):
                                kc = g4 * 4 + i4
                                op("pe", lambda e, kc=kc, i4=i4: e.transpose(pb[bk][:, i4 * 128:(i4 + 1) * 128], xin[xi][:, kc * 128:(kc + 1) * 128], ident[:]),
                                   reads=["xin%d" % xi, "ident"], writes=[PB[bk]], inc=(i4 == 3))
                            dst = xT[b][:, g4 * 4:(g4 + 1) * 4, tt * 128:(tt + 1) * 128]
                            src = pb[bk][:].rearrange("p (a b) -> p a b", b=128)
                            if g4 % 2 == 0:
                                op("act", lambda e: e.activation(out=dst, in_=src, func=AF.Copy), reads=[PB[bk]], writes=["xT%d" % b])
                            else:
                                op("dve", lambda e: e.tensor_copy(out=dst, in_=src), reads=[PB[bk]], writes=["xT%d" % b])

                def inproj(n, bank, ncols=CH, c0=0):
                    ci, j, kind, idx = items[n]
                    s = n % 3
                    b = ci % 2
                    for kc in range(16):
                        op("pe", lambda e, kc=kc: e.matmul(pb[bank][:, 0:ncols], wbf[s][:, kc, :], xT[b][:, kc, c0:c0 + ncols],
                                                          start=(kc == 0), stop=(kc == 15)),
                           reads=["wbf%d" % s, "xT%d" % b], writes=[PB[bank]], inc=(kc == 15))

                def ssq_accum(ytile_key, ysrc, first, last, gidx=1):
                    if gidx == 1:
                        op("act", lambda e: e.activation(out=Wb[2][:, 0, :], in_=ysrc, func=AF.Square), reads=[ytile_key], writes=["Wb2_0"])
                        op("pe", lambda e: e.matmul(pb[6][:], ones_b[:], Wb[2][:, 0, :], start=first, stop=last),
                           reads=["Wb2_0", "ones_b"], writes=[PB[6]], inc=True)
                        return
                    vk = "vg%d" % gidx
                    if first:
                        op("act", lambda e: e.activation(out=vg[:, gidx, :], in_=ysrc, func=AF.Square), reads=[ytile_key], writes=[vk])
                    else:
                        op("act", lambda e: e.activation(out=W[6][:, 0:CH], in_=ysrc, func=AF.Square), reads=[ytile_key], writes=["W6"])
                        op("pool", lambda e: e.tensor_tensor(out=vg[:, gidx, :], in0=vg[:, gidx, :], in1=W[6][:, 0:CH], op=ALU.add),
                           reads=[vk, "W6"], writes=[vk])
                    if last:
                        op("pe", lambda e: e.matmul(pb[4][:], ones_f[:], vg[:, gidx, :], start=True, stop=True),
                           reads=["ones_f", vk], writes=[PB[4]])

                def group_var(gidx, n_ch):
                    bank = 6 if gidx == 1 else 4
                    op("dve", lambda e: e.tensor_scalar(out=vg[:, gidx, :], in0=pb[bank][:], scalar1=1.0 / n_ch, scalar2=EPS, op0=ALU.mult, op1=ALU.add),
                       reads=[PB[bank]], writes=["vg%d" % gidx])

                setof = {}

                def ubank(n):
                    return (2, 3, 6)[n % 3] if not own_phase else 2 + (n % 2)

                def lru_s1(n):
                    lru_s1a(n)
                    lru_s1b(n)

                def lru_s1a(n):
                    ci, j, kind, blk = items[n]
                    bank = ubank(n)
                    inproj(n, bank)
                    ue = "uext%d" % blk
                    op("dve", lambda e: e.tensor_copy(out=uext[:, blk, 0:3], in_=uext[:, blk, CH:CH + 3]), reads=[ue], writes=[ue])
                    op("dve", lambda e: e.tensor_copy(out=uext[:, blk, 3:CH + 3], in_=pb[bank][:]), reads=[PB[bank]], writes=[ue])

                def lru_s1b(n):
                    ci, j, kind, blk = items[n]
                    st = setof[n]
                    ucb = ucb_aps[st]
                    ucbk = "ucb%d" % st
                    bank = ubank(n)
                    ue = "uext%d" % blk
                    for k in range(4):
                        op("pe", lambda e, k=k: e.matmul(pb[bank][:], dg[:, blk, k, :], uext[:, blk, k:k + CH], start=(k == 0), stop=(k == 3)),
                           reads=["dg", ue], writes=[PB[bank]], inc=(k == 3))
                    op("act", lambda e: e.activation(out=ucb, in_=pb[bank][:], func=AF.Identity, bias=chv[:, O_CB + blk:O_CB + blk + 1]),
                       reads=[PB[bank], "chv"], writes=[ucbk])

                def lru_s2(n):
                    ci, j, kind, blk = items[n]
                    st = setof[n]
                    AK = akeys(st)
                    acc, thr, thi, aa, a2 = A[st]
                    ucb = ucb_aps[st]
                    ucbk = "ucb%d" % st
                    pr_, pi_ = (4, 5) if st % 2 == 0 else (1, 7)
                    op("pe", lambda e: e.matmul(pb[pr_][:], wa_b[:, blk * 128:(blk + 1) * 128], ucb, start=True, stop=True),
                       reads=["wa_b", ucbk], writes=[PB[pr_]])
                    op("pe", lambda e: e.matmul(pb[pi_][:], wx_b[:, blk * 128:(blk + 1) * 128], ucb, start=True, stop=True),
                       reads=["wx_b", ucbk], writes=[PB[pi_]])
                    op("act", lambda e: e.activation(out=thr[:], in_=pb[pr_][:], func=AF.Tanh, scale=0.5, bias=cl[:, 16 + blk:17 + blk]),
                       reads=[PB[pr_], "cl"], writes=[AK[1]])
                    op("act", lambda e: e.activation(out=thi[:], in_=pb[pi_][:], func=AF.Tanh, scale=0.5, bias=cl[:, 24 + blk:25 + blk]),
                       reads=[PB[pi_], "cl"], writes=[AK[2]])
                    op("act", lambda e: e.activation(out=aa[:], in_=thr[:], func=AF.Exp, scale=cl[:, blk:blk + 1], bias=cl[:, blk:blk + 1]),
                       reads=[AK[1], "cl"], writes=[AK[3]])
                    op("act", lambda e: e.activation(out=a2[:], in_=thr[:], func=AF.Exp, scale=cl[:, 8 + blk:9 + blk], bias=cl[:, 8 + blk:9 + blk]),
                       reads=[AK[1], "cl"], writes=[AK[4]])

                def lru_s3(n):
                    ci, j, kind, blk = items[n]
                    st = setof[n]
                    AK = akeys(st)
                    acc, thr, thi, aa, a2 = A[st]
                    ucb = ucb_aps[st]
                    ucbk = "ucb%d" % st
                    op("dve", lambda e: e.tensor_scalar_min(out=a2[:], in0=a2[:], scalar1=0.9999999), reads=[AK[4]], writes=[AK[4]])
                    op("dve", lambda e: e.scalar_tensor_tensor(out=thi[:], in0=thi[:], scalar=1.0, in1=ucb, op0=ALU.add, op1=ALU.mult),
                       reads=[AK[2], ucbk], writes=[AK[2]])

                def lru_s4a(n):
                    ci, j, kind, blk = items[n]
                    st = setof[n]
                    AK = akeys(st)
                    acc, thr, thi, aa, a2 = A[st]
                    op("act", lambda e: e.activation(out=a2[:], in_=a2[:], func=AF.Sqrt, scale=-1.0, bias=1.0), reads=[AK[4]], writes=[AK[4]])

                def lru_s4b(n):
                    ci, j, kind, blk = items[n]
                    st = setof[n]
                    AK = akeys(st)
                    acc, thr, thi, aa, a2 = A[st]
                    op("dve", lambda e: e.scalar_tensor_tensor(out=a2[:], in0=a2[:], scalar=vmh[:, ci:ci + 1], in1=thi[:], op0=ALU.mult, op1=ALU.mult),
                       reads=[AK[4], AK[2], "vmh"], writes=[AK[4]])
                    op("dve", lambda e: e.tensor_tensor_scan(out=acc[:], data0=aa[:], data1=a2[:], initial=hst[:, blk:blk + 1],
                                                             op0=ALU.mult, op1=ALU.add),
                       reads=[AK[3], AK[4], "hst"], writes=[AK[0]])
                    op("pool", lambda e: e.tensor_copy(out=hst[:, blk:blk + 1], in_=acc[:, CH - 1:CH]), reads=[AK[0]], writes=["hst"])

                def lru_g(n, do_inproj=True):
                    ci, j, kind, blk = items[n]
                    st = blk % 2
                    hk = "A%d_0" % st
                    oc = ci - NPRE
                    bank = 2 + (n % 2)
                    if do_inproj:
                        inproj(n, bank)
                    gg = W[6]
                    op("act", lambda e: e.activation(out=gg[:, 0:CH], in_=pb[bank][:], func=AF.Gelu_apprx_tanh), reads=[PB[bank]], writes=["W6"])
                    ysl = yT[:, 4 + blk, oc * CH:(oc + 1) * CH]
                    yk = "yT%d" % (4 + blk)
                    op("dve", lambda e: e.tensor_tensor(out=ysl, in0=gg[:, 0:CH], in1=A[st][0][:], op=ALU.mult), reads=["W6", hk], writes=[yk])
                    ssq_accum(yk, ysl, blk == 0, blk == 7)
                    if blk == 7:
                        group_var(1, 1024.0)

                def pool_blk(n, halo_only=False, do_inproj=True):
                    ci, j, kind, gi = items[n]
                    bank = 2 + (n % 2)
                    pk = "upx%d" % gi
                    if halo_only:
                        inproj(n, bank, ncols=16, c0=CH - 16)
                        op("act", lambda e: e.activation(out=upx[:, gi, CH:CH + 16], in_=pb[bank][:, 0:16], func=AF.Copy), reads=[PB[bank]], writes=[pk])
                        return
                    oc = ci - NPRE
                    if do_inproj:
                        inproj(n, bank)
                    op("dve", lambda e: e.tensor_copy(out=upx[:, gi, 0:16], in_=upx[:, gi, CH:CH + 16]), reads=[pk], writes=[pk])
                    op("act", lambda e: e.activation(out=upx[:, gi, 16:CH + 16], in_=pb[bank][:], func=AF.Copy), reads=[PB[bank]], writes=[pk])
                    src = upx[:, gi, :]
                    srck = pk
                    step = 1
                    lo = 0
                    for lvl in range(gi + 1):
                        dstt = W[7 + (lvl % 2)]
                        dk = "W%d" % (7 + (lvl % 2))
                        lo2 = lo + step
                        op("pool", lambda e, src=src, dstt=dstt, lo2=lo2, step=step: e.tensor_tensor(out=dstt[:, lo2:CH + 16], in0=src[:, lo2:CH + 16], in1=src[:, lo2 - step:CH + 16 - step], op=ALU.add),
                           reads=[srck], writes=[dk])
                        src, srck, lo, step = dstt, dk, lo2, step * 2
                    wlen = float(2 ** (gi + 1))
                    dbf = Wb[1][:, 0, :]
                    op("dve", lambda e: e.scalar_tensor_tensor(out=dbf, in0=src[:, 16:CH + 16], scalar=1.0 / wlen, in1=upx[:, gi, 16:CH + 16], op0=ALU.mult, op1=ALU.subtract),
                       reads=[srck, pk], writes=["Wb1_0"])
                    if oc == 0:
                        tmp = W[6]
                        op("dve", lambda e: e.tensor_tensor(out=tmp[:, 0:16], in0=src[:, 16:32], in1=invc[:, gi * 16:(gi + 1) * 16], op=ALU.mult),
                           reads=[srck, "invc"], writes=["W6"])
                        op("dve", lambda e: e.tensor_tensor(out=Wb[1][:, 0, 0:16], in0=tmp[:, 0:16], in1=upx[:, gi, 16:32], op=ALU.subtract),
                           reads=["W6", pk, "Wb1_0"], writes=["Wb1_0"])
                    op("pe", lambda e: e.matmul(pb[4][:], wpool_b[:, gi * 128:(gi + 1) * 128], dbf, start=True, stop=True),
                       reads=["wpool_b", "Wb1_0"], writes=[PB[4]])
                    ysl = yT[:, gi, oc * CH:(oc + 1) * CH]
                    yk = "yT%d" % gi
                    op("act", lambda e: e.activation(out=ysl, in_=pb[4][:], func=AF.Identity, scale=chv[:, O_PS + gi:O_PS + gi + 1]),
                       reads=[PB[4], "chv"], writes=[yk])
                    ssq_accum(yk, ysl, gi == 0, gi == 3, gidx=0)
                    if gi == 3:
                        group_var(0, 512.0)

                def mem_blk(n, do_inproj=True):
                    ci, j, kind, h = items[n]
                    oc = ci - NPRE
                    bank = 2 + (n % 2)
                    if do_inproj:
                        inproj(n, bank)
                    qT = Wb[1][:, 1, :]
                    op("act", lambda e: e.activation(out=qT, in_=pb[bank][:], func=AF.Copy, scale=128.0 ** -0.5), reads=[PB[bank]], writes=["Wb1_1"])
                    for mc in range(2):
                        op("pe", lambda e, mc=mc: e.matmul(pb[4 + mc][:], kT[:, h, mc * 128:(mc + 1) * 128], qT, start=True, stop=True),
                           reads=["kT", "Wb1_1"], writes=[PB[4 + mc]])
                    for mc in range(2):
                        op("act", lambda e, mc=mc: e.activation(out=ucbs[:, mc, :], in_=pb[4 + mc][:], func=AF.Exp), reads=[PB[4 + mc]], writes=["ucb%d" % mc])
                    for mc in range(2):
                        op("pe", lambda e, mc=mc: e.matmul(pb[4][:], vv[:, mc, h * 128:(h + 1) * 128], ucbs[:, mc, :], start=(mc == 0), stop=(mc == 1)),
                           reads=["vv", "ucb%d" % mc], writes=[PB[4]], inc=(mc == 1))
                    for mc in range(2):
                        op("pe", lambda e, mc=mc: e.matmul(pb[5][:], ones_b[:], ucbs[:, mc, :], start=(mc == 0), stop=(mc == 1)),
                           reads=["ones_b", "ucb%d" % mc], writes=[PB[5]], inc=(mc == 1))
                    rinv = W[6]
                    op("dve", lambda e: e.reciprocal(out=rinv[:, 0:CH], in_=pb[5][:]), reads=[PB[5]], writes=["W6"])
                    ysl = yT[:, 12 + h, oc * CH:(oc + 1) * CH]
                    yk = "yT%d" % (12 + h)
                    op("dve", lambda e: e.tensor_tensor(out=ysl, in0=pb[4][:], in1=rinv[:, 0:CH], op=ALU.mult), reads=[PB[4], "W6"], writes=[yk])
                    ssq_accum(yk, ysl, h == 0, h == 3, gidx=2)
                    if h == 3:
                        group_var(2, 512.0)

                def finish_chunk(ci):
                    oc = ci - NPRE
                    for gidx in range(3):
                        op("act", lambda e, gidx=gidx: e.activation(out=vg[:, gidx, :], in_=vg[:, gidx, :], func=AF.Sqrt), reads=["vg%d" % gidx], writes=["vg%d" % gidx])
                    for gidx in range(3):
                        op("dve", lambda e, gidx=gidx: e.reciprocal(out=vg[:, gidx, :], in_=vg[:, gidx, :]), reads=["vg%d" % gidx], writes=["vg%d" % gidx])
                    for blk16 in range(16):
                        gidx = 0 if blk16 < 4 else (1 if blk16 < 12 else 2)
                        ysl = yT[:, blk16, oc * CH:(oc + 1) * CH]
                        eng = "dve" if blk16 % 2 == 0 else "pool"
                        op(eng, lambda e, ysl=ysl, gidx=gidx: e.tensor_tensor(out=ysl, in0=ysl, in1=vg[:, gidx, :], op=ALU.mult),
                           reads=["yT%d" % blk16, "vg%d" % gidx], writes=["yT%d" % blk16])

                def prefetch(n):
                    if n + 2 < NI:
                        load_w(n + 2)

                prep_chunk(chunks[0])
                load_w(0)
                if NI > 1:
                    load_w(1)
                if not own_phase:
                    GS = NS // 2
                    us = [n for n in range(NI) if items[n][2] == "u"]
                    rest = [n for n in range(NI) if items[n][2] != "u"]
                    upairs = [tuple(us[i:i + GS]) for i in range(0, len(us), GS)]
                    for g_, grp in enumerate(upairs):
                        for ii, n in enumerate(grp):
                            setof[n] = (g_ % 2) * GS + ii
                    G = len(upairs)
                    for g_ in range(G + 1):
                        prev = upairs[g_ - 1] if g_ >= 1 else None
                        cur = upairs[g_] if g_ < G else None
                        for i in range(GS + 1):
                            if cur and i < GS:
                                prefetch(cur[i])
                                lru_s1a(cur[i])
                                ci, j, kind, idx = items[cur[i]]
                                if i == 0 and idx == 0 and ci + 1 <= chunks[-1]:
                                    prep_chunk(ci + 1)
                            if cur and i >= 1:
                                lru_s1b(cur[i - 1])
                            if prev and i >= 1:
                                lru_s2(prev[i - 1])
                                lru_s3(prev[i - 1])
                        if prev:
                            for n in prev:
                                lru_s4a(n)
                            for n in prev:
                                lru_s4b(n)
                    for n in rest:
                        prefetch(n)
                        pool_blk(n, halo_only=True)
                else:
                    def ip(m):
                        prefetch(m)
                        inproj(m, 2 + (m % 2))

                    n = 0
                    while n < NI:
                        ci, j, kind, idx = items[n]
                        na, nb, np_, nga, ngb, nm = n, n + 1, n + 2, n + 3, n + 4, n + 5
                        setof[na], setof[nb] = 0, 1
                        for m in (na, nb):
                            prefetch(m)
                            lru_s1a(m)
                        if idx == 0 and ci + 1 <= chunks[-1]:
                            prep_chunk(ci + 1)
                        for m in (na, nb):
                            lru_s1b(m)
                        ip(np_)
                        for m in (na, nb):
                            lru_s2(m)
                        ip(nga)
                        pool_blk(np_, do_inproj=False)
                        for m in (na, nb):
                            lru_s3(m)
                        for m in (na, nb):
                            lru_s4a(m)
                        for m in (na, nb):
                            lru_s4b(m)
                        lru_g(nga, do_inproj=False)
                        ip(ngb)
                        ip(nm)
                        lru_g(ngb, do_inproj=False)
                        mem_blk(nm, do_inproj=False)
                        last_of_chunk = (nm + 1 == NI) or (items[nm + 1][0] != ci)
                        if last_of_chunk:
                            finish_chunk(ci)
                        n += 6
                P.barrier()

            phase1(list(range(0, NPRE)), 8, False)
            phase1(list(range(NPRE, NCH)), 2, True)
            sp1.close()

            with ExitStack() as s2:
                woutg = sbin(s2, "woutg", [128, 16, D], BF16)
                with ExitStack() as s2a:
                    wos = [sbin(s2a, "wos%d" % i, [128, D], F32) for i in range(2)]
                    for kb in range(16):
                        s = kb % 2
                        dma("sp", lambda e, kb=kb, s=s: e.dma_start(out=wos[s][:], in_=w_out_t[:, kb, :]), writes=["wos%d" % s])
                        op("dve",
                           lambda e, kb=kb, s=s: e.tensor_scalar(out=woutg[:, kb, :], in0=wos[s][:], scalar1=chv[:, O_G + kb:O_G + kb + 1], scalar2=None, op0=ALU.mult),
                           reads=["wos%d" % s, "chv"], writes=["woutg"])
                    P.barrier()
                xin2 = [sbin(s2, "xin2_%d" % i, [128, D], F32) for i in range(2)]
                Z = [sbin(s2, "Z%d" % i, [128, D], F32) for i in range(2)]
                x1T = sbin(s2, "x1T", [128, 16, 128], F32)
                lng = sbin(s2, "lng", [128, D], F32)
                lnb = sbin(s2, "lnb", [128, D], F32)
                stats2 = [sbin(s2, "stats_%d" % i, [128, 4, 6], F32) for i in range(2)]
                dma("sp", lambda e: e.dma_start(out=lng[:], in_=lnp[0:1, :].partition_broadcast(128)), writes=["lng"])
                dma("sp", lambda e: e.dma_start(out=lnb[:], in_=lnp[1:2, :].partition_broadcast(128)), writes=["lnb"])
                def p2_load(t):
                    zi = t % 2
                    r0 = NPRE * CH + t * 128
                    dma("sp", lambda e: e.dma_start(out=xin2[zi][:], in_=xe[r0:r0 + 128, :]), writes=["xin2_%d" % zi])

                def p2_mm(t, cs):
                    for c in cs:
                        bank = 2 + (c % 2)
                        for kb in range(16):
                            op("pe", lambda e, kb=kb, c=c, bank=bank: e.matmul(pb[bank][:], yT[:, kb, t * 128:(t + 1) * 128], woutg[:, kb, c * 512:(c + 1) * 512],
                                                                              start=(kb == 0), stop=(kb == 15)),
                               reads=["yT", "woutg"], writes=[PB[bank]], inc=(kb == 15))

                def p2_evac(t, cs):
                    zi = t % 2
                    zk = "Z%d" % zi
                    xk = "xin2_%d" % zi
                    for c in cs:
                        bank = 2 + (c % 2)
                        op("dve", lambda e, c=c, bank=bank: e.scalar_tensor_tensor(out=Z[zi][:, c * 512:(c + 1) * 512], in0=xin2[zi][:, c * 512:(c + 1) * 512], scalar=ALPHA,
                                                                                   in1=pb[bank][:], op0=ALU.mult, op1=ALU.add),
                           reads=[xk, PB[bank]], writes=[zk])
                        op("dve", lambda e, c=c: e.bn_stats(out=stats2[zi][:, c, :], in_=Z[zi][:, c * 512:(c + 1) * 512]), reads=[zk], writes=["stats_%d" % zi])

                def p2_back(t):
                    zi = t % 2
                    zk = "Z%d" % zi
                    nxt = t + 1 if t + 1 < 16 else None
                    layer_norm_tail(P, zk, Z[zi], stats2[zi], sm, lng, lnb, "lng", "lnb", stats_key="stats_%d" % zi)
                    if nxt is not None:
                        p2_load(nxt)
                    for g4 in range(4):
                        bk = 4 + g4
                        for i4 in range(4):
                            kc = g4 * 4 + i4
                            op("pe", lambda e, kc=kc, i4=i4, bk=bk: e.transpose(pb[bk][:, i4 * 128:(i4 + 1) * 128], Z[zi][:, kc * 128:(kc + 1) * 128], ident[:]),
                               reads=[zk, "ident"], writes=[PB[bk]], inc=(i4 == 3))
                        op("act", lambda e, g4=g4, bk=bk: e.activation(out=x1T[:, g4 * 4:(g4 + 1) * 4, :], in_=pb[bk][:].rearrange("p (a b) -> p a b", b=128), func=AF.Copy),
                           reads=[PB[bk]], writes=["x1T"])
                    for kc in range(16):
                        op("pe", lambda e, kc=kc: e.matmul(pb[0][:, 0:36], x1T[:, kc, :], w_r[:, kc, :], start=(kc == 0), stop=(kc == 15)),
                           reads=["x1T", "w_r"], writes=[PB[0]], inc=(kc == 15))
                    route_tile(P, t, pb, PB, sm, sm32, b_rb, rcb, sbase, ltri, ones_f, dest_i, w12,
                               pre_pos_hook=(lambda: p2_mm(nxt, (0, 1))) if nxt is not None else None,
                               mid_hook=(lambda: p2_evac(nxt, (0, 1))) if nxt is not None else None)
                    if nxt is not None:
                        p2_mm(nxt, (2, 3))
                        p2_evac(nxt, (2, 3))
                    for k in range(2):
                        col = 2 * t + k
                        dma("pool", lambda e, col=col: e.indirect_dma_start(out=Xs[:, :], out_offset=bass.IndirectOffsetOnAxis(ap=dest_i[:, col:col + 1], axis=0),
                                                                            in_=Z[zi][:, :], in_offset=None, bounds_check=NE * CAP - 1, oob_is_err=False),
                            reads=[zk, "dest_i"], writes=["Xs"])
                    dma("pool", lambda e: e.dma_start(out=X1s[t * 128:(t + 1) * 128, :], in_=Z[zi][:]), reads=[zk], writes=["X1s"])
                    if DEBUG:
                        dma("sp", lambda e: e.dma_start(out=dbg_x1[t * 128:(t + 1) * 128, :], in_=Z[zi][:]), reads=[zk], writes=["dbg_x1"])
                p2_load(0)
                p2_mm(0, (0, 1))
                p2_evac(0, (0, 1))
                p2_mm(0, (2, 3))
                p2_evac(0, (2, 3))
                for t in range(16):
                    p2_back(t)
                P.barrier()

        with ExitStack() as s3:
            ring = [sbin(s3, "ring%d" % i, [128, 16 * 512], F32R) for i in range(3)]
            xs = [sbin(s3, "xs%d" % i, [128, D], F32) for i in range(2)]
            XT = [sbin(s3, "XT%d" % i, [128, 16, CAP], F32R) for i in range(2)]
            hT = sbin(s3, "hT", [128, 8, CAP], F32R)
            sg = [sbin(s3, "sg%d" % i, [128, CAP], F32) for i in range(2)]
            yo = [sbin(s3, "yo%d" % i, [128, 1024], F32) for i in range(3)]
            op("pool", lambda e: e.memset(sg[0][0:1, :], 0.0), writes=["sg0"])
            for cz in range(D // CAP + 1):
                w_ = min(CAP, D - cz * CAP)
                if w_ <= 0:
                    break
                dma("pool", lambda e, cz=cz, w_=w_: e.dma_start(out=Ys[NE * CAP:NE * CAP + 1, cz * CAP:cz * CAP + w_], in_=sg[0][0:1, 0:w_]),
                    reads=["sg0"], writes=["Ys"])
            pieces = []
            for e_ in range(NE):
                for n_ in range(2):
                    pieces.append((e_, "g", n_))
                    pieces.append((e_, "u", n_))
                for h_ in range(2):
                    pieces.append((e_, "d", h_))
            NP_ = len(pieces)
            yo_ctr = [0]

            def load_piece(i):
                e_, kind, n_ = pieces[i]
                r = i % 3
                src = {"g": wg_t, "u": wu_t, "d": wd_t}[kind]
                dma("sp", lambda e: e.dma_start(out=ring[r][:], in_=src[e_, n_]), writes=["ring%d" % r])

            def prep_expert(e_):
                b = e_ % 2
                for s in range(NSB):
                    xi = s % 2
                    r0 = e_ * CAP + s * 128
                    dma("sp", lambda e, xi=xi, r0=r0: e.dma_start(out=xs[xi][:], in_=Xs[r0:r0 + 128, :]), reads=["Xs"], writes=["xs%d" % xi])
                    for g4 in range(4):
                        bk = g4 % 2
                        for i4 in range(4):
                            kc = g4 * 4 + i4
                            op("pe", lambda e, xi=xi, kc=kc, i4=i4, bk=bk: e.transpose(pb[bk][:, i4 * 128:(i4 + 1) * 128], xs[xi][:, kc * 128:(kc + 1) * 128], ident[:]),
                               reads=["xs%d" % xi, "ident"], writes=[PB[bk]], inc=(i4 == 3))
                        op("act" if g4 % 2 == 0 else "dve",
                           (lambda e, s=s, g4=g4, bk=bk: e.activation(out=XT[b][:, g4 * 4:(g4 + 1) * 4, s * 128:(s + 1) * 128], in_=pb[bk][:].rearrange("p (a b) -> p a b", b=128), func=AF.Copy))
                           if g4 % 2 == 0 else
                           (lambda e, s=s, g4=g4, bk=bk: e.tensor_copy(out=XT[b][:, g4 * 4:(g4 + 1) * 4, s * 128:(s + 1) * 128], in_=pb[bk][:].rearrange("p (a b) -> p a b", b=128))),
                           reads=[PB[bk]], writes=["XT%d" % b])

            load_piece(0)
            load_piece(1)
            prep_expert(0)
            for i in range(NP_):
                e_, kind, n_ = pieces[i]
                b = e_ % 2
                r = i % 3
                if kind == "g":
                    if i + 2 < NP_:
                        load_piece(i + 2)
                    continue
                if kind == "u":
                    rg = (i - 1) % 3
                    for f in range(4):
                        fc = n_ * 4 + f
                        bg = 2 + (fc % 2)
                        bu = 4 + (fc % 2)
                        for kc in range(16):
                            op("pe", lambda e, kc=kc, f=f, bg=bg, rg=rg: e.matmul(pb[bg][:, 0:CAP], ring[rg][:, kc * 512 + f * 128:kc * 512 + (f + 1) * 128], XT[b][:, kc, :],
                                                                                  start=(kc == 0), stop=(kc == 15)),
                               reads=["ring%d" % rg, "XT%d" % b], writes=[PB[bg]], inc=(kc == 15))
                        for kc in range(16):
                            op("pe", lambda e, kc=kc, f=f, bu=bu: e.matmul(pb[bu][:, 0:CAP], ring[r][:, kc * 512 + f * 128:kc * 512 + (f + 1) * 128], XT[b][:, kc, :],
                                                                          start=(kc == 0), stop=(kc == 15)),
                               reads=["ring%d" % r, "XT%d" % b], writes=[PB[bu]], inc=(kc == 15))
                        si = fc % 2
                        op("act", lambda e, bg=bg, si=si: e.activation(out=sg[si][:], in_=pb[bg][:, 0:CAP], func=AF.Silu), reads=[PB[bg]], writes=["sg%d" % si])
                        op("dve", lambda e, bu=bu, si=si, fc=fc: e.tensor_tensor(out=hT[:, fc, :], in0=sg[si][:], in1=pb[bu][:, 0:CAP], op=ALU.mult),
                           reads=["sg%d" % si, PB[bu]], writes=["hT"])
                    if n_ == 1 and e_ + 1 < NE:
                        prep_expert(e_ + 1)
                else:
                    for s in range(NSB):
                        yi = yo_ctr[0] % 3
                        yo_ctr[0] += 1
                        for c in range(2):
                            bank = 6 + c
                            for fc in range(8):
                                op("pe", lambda e, fc=fc, s=s, c=c, bank=bank: e.matmul(pb[bank][:], hT[:, fc, s * 128:(s + 1) * 128], ring[r][:, fc * 1024 + c * 512:fc * 1024 + (c + 1) * 512],
                                                                                      start=(fc == 0), stop=(fc == 7)),
                                   reads=["hT", "ring%d" % r], writes=[PB[bank]], inc=(fc == 7))
                            if c == 0:
                                op("act", lambda e, yi=yi, bank=bank, c=c: e.activation(out=yo[yi][:, c * 512:(c + 1) * 512], in_=pb[bank][:], func=AF.Copy),
                                   reads=[PB[bank]], writes=["yo%d" % yi])
                            else:
                                op("dve", lambda e, yi=yi, bank=bank, c=c: e.tensor_copy(out=yo[yi][:, c * 512:(c + 1) * 512], in_=pb[bank][:]),
                                   reads=[PB[bank]], writes=["yo%d" % yi])
                        r0 = e_ * CAP + s * 128
                        dma("pool", lambda e, yi=yi, r0=r0, n_=n_: e.dma_start(out=Ys[r0:r0 + 128, n_ * 1024:(n_ + 1) * 1024], in_=yo[yi][:]),
                            reads=["yo%d" % yi], writes=["Ys"])
                if i + 2 < NP_:
                    load_piece(i + 2)
            P.barrier()

        with ExitStack() as s4:
            y1 = [sbin(s4, "y1_%d" % i, [128, D], F32) for i in range(3)]
            y2 = [sbin(s4, "y2_%d" % i, [128, D], F32) for i in range(3)]
            xr = [sbin(s4, "xr_%d" % i, [128, D], F32) for i in range(3)]
            lng = sbin(s4, "lng2", [128, D], F32)
            lnb = sbin(s4, "lnb2", [128, D], F32)
            stats3 = [sbin(s4, "stats3_%d" % i, [128, 4, 6], F32) for i in range(3)]
            dma("sp", lambda e: e.dma_start(out=lng[:], in_=lnp[2:3, :].partition_broadcast(128)), writes=["lng2"])
            dma("sp", lambda e: e.dma_start(out=lnb[:], in_=lnp[3:4, :].partition_broadcast(128)), writes=["lnb2"])
            def c_loads(t):
                i = t % 3
                c1, c2 = 2 * t, 2 * t + 1
                dma("pool", lambda e, i=i, c1=c1: e.indirect_dma_start(out=y1[i][:, :], out_offset=None, in_=Ys[:, :],
                                                                      in_offset=bass.IndirectOffsetOnAxis(ap=dest_i[:, c1:c1 + 1], axis=0)),
                    reads=["Ys", "dest_i"], writes=["y1_%d" % i])
                dma("pool", lambda e, i=i, c2=c2: e.indirect_dma_start(out=y2[i][:, :], out_offset=None, in_=Ys[:, :],
                                                                      in_offset=bass.IndirectOffsetOnAxis(ap=dest_i[:, c2:c2 + 1], axis=0)),
                    reads=["Ys", "dest_i"], writes=["y2_%d" % i])
                dma("pool", lambda e, i=i, t=t: e.dma_start(out=xr[i][:], in_=X1s[t * 128:(t + 1) * 128, :]), reads=["X1s"], writes=["xr_%d" % i])

            def c_front(t):
                i = t % 3
                c1, c2 = 2 * t, 2 * t + 1
                ak = "xr_%d" % i
                acc = xr[i]
                op("act", lambda e: e.activation(out=acc[:], in_=acc[:], func=AF.Copy, scale=ALPHA), reads=[ak], writes=[ak])
                op("dve", lambda e: e.scalar_tensor_tensor(out=acc[:], in0=y1[i][:], scalar=w12[:, c1:c1 + 1], in1=acc[:], op0=ALU.mult, op1=ALU.add),
                   reads=["y1_%d" % i, "w12", ak], writes=[ak])
                op("dve", lambda e: e.scalar_tensor_tensor(out=acc[:], in0=y2[i][:], scalar=w12[:, c2:c2 + 1], in1=acc[:], op0=ALU.mult, op1=ALU.add),
                   reads=["y2_%d" % i, "w12", ak], writes=[ak])
                for c in range(4):
                    op("dve", lambda e, c=c: e.bn_stats(out=stats3[i][:, c, :], in_=acc[:, c * 512:(c + 1) * 512]), reads=[ak], writes=["stats3_%d" % i])

            def c_back(t):
                i = t % 3
                ak = "xr_%d" % i
                acc = xr[i]
                layer_norm_tail(P, ak, acc, stats3[i], sm, lng, lnb, "lng2", "lnb2", stats_key="stats3_%d" % i)
                dma("sp", lambda e: e.dma_start(out=out[t * 128:(t + 1) * 128, :], in_=acc[:]), reads=[ak], writes=["out"])

            c_loads(0)
            c_loads(1)
            c_front(0)
            for t in range(16):
                if t + 2 < 16:
                    c_loads(t + 2)
                if t + 1 < 16:
                    c_front(t + 1)
                c_back(t)
            if DEBUG:
                dma("sp", lambda e: e.dma_start(out=dbg_rt[0:128, 0:4], in_=w12[:, 0:4]), reads=["w12"], writes=["dbg_rt"])
            P.finish("sp")
            P.finish("pool")
        print("n_inst", P.n_inst, "sems", 4 + len(P.dsem))
    return nc


def layer_norm_tail(P, zk, Zt, stats, sm, lng, lnb, gk, bk_, stats_key="stats"):
    op = P.op
    op("dve", lambda e: e.bn_aggr(out=sm[:, 16:18], in_=stats[:].rearrange("p a b -> p (a b)")), reads=[stats_key], writes=["sm_mv"])
    op("dve", lambda e: e.tensor_scalar(out=sm[:, 18:19], in0=sm[:, 17:18], scalar1=EPS, scalar2=None, op0=ALU.add), reads=["sm_mv"], writes=["sm_r"])
    op("act", lambda e: e.activation(out=sm[:, 18:19], in_=sm[:, 18:19], func=AF.Sqrt), reads=["sm_r"], writes=["sm_r"])
    op("dve", lambda e: e.reciprocal(out=sm[:, 18:19], in_=sm[:, 18:19]), reads=["sm_r"], writes=["sm_r"])
    op("dve", lambda e: e.scalar_tensor_tensor(out=sm[:, 19:20], in0=sm[:, 16:17], scalar=-1.0, in1=sm[:, 18:19], op0=ALU.mult, op1=ALU.mult),
       reads=["sm_mv", "sm_r"], writes=["sm_n"])
    op("act", lambda e: e.activation(out=Zt[:], in_=Zt[:], func=AF.Identity, scale=sm[:, 18:19], bias=sm[:, 19:20]),
       reads=[zk, "sm_r", "sm_n"], writes=[zk])
    op("dve", lambda e: e.tensor_tensor(out=Zt[:], in0=Zt[:], in1=lng[:], op=ALU.mult), reads=[zk, gk], writes=[zk])
    op("dve", lambda e: e.tensor_tensor(out=Zt[:], in0=Zt[:], in1=lnb[:], op=ALU.add), reads=[zk, bk_], writes=[zk])


def route_tile(P, t, pb, PB, sm, sm32, b_rb, rcb, sbase, ltri, ones_f, dest_i, w12, pre_pos_hook=None, mid_hook=None):
    op = P.op
    L = sm32[:, 0, :]
    fm = sm32[:, 1, 0:32]
    eq1 = sm32[:, 2, 0:32]
    eq2 = sm32[:, 3, 0:32]
    oh = sm32[:, 4, 0:32]
    sl = sm32[:, 5, 0:32]
    tmp = sm32[:, 6, 0:32]
    top8 = sm32[:, 7, 0:8]
    K = "rt"
    op("dve", lambda e: e.tensor_tensor(out=L, in0=pb[0][:, 0:36], in1=b_rb[:], op=ALU.add), reads=[PB[0], "b_rb"], writes=[K])
    gmax, ngmax, gsum, gp = sm[:, 20:21], sm[:, 21:22], sm[:, 22:23], sm[:, 23:24]
    op("dve", lambda e: e.reduce_max(out=gmax, in_=L[:, 0:4], axis=AX.X), reads=[K], writes=["rt_a"])
    op("dve", lambda e: e.tensor_scalar(out=ngmax, in0=gmax, scalar1=-1.0, scalar2=None, op0=ALU.mult), reads=["rt_a"], writes=["rt_b"])
    op("act", lambda e: e.activation(out=sm[:, 24:28], in_=L[:, 0:4], func=AF.Exp, bias=ngmax, accum_out=gsum), reads=[K, "rt_b"], writes=["rt_c"])
    op("dve", lambda e: e.reciprocal(out=gp, in_=gsum), reads=["rt_c"], writes=["rt_d"])
    pen = sm[:, 28:32]
    op("dve", lambda e: e.tensor_scalar(out=pen, in0=L[:, 0:4], scalar1=gmax, scalar2=None, op0=ALU.is_equal), reads=[K, "rt_a"], writes=["rt_e"])
    op("dve", lambda e: e.tensor_scalar(out=pen, in0=pen, scalar1=-1.0, scalar2=1e30, op0=ALU.add, op1=ALU.mult), reads=["rt_e"], writes=["rt_e"])
    for gi in range(4):
        op("dve", lambda e, gi=gi: e.tensor_scalar(out=fm[:, gi * 8:(gi + 1) * 8], in0=L[:, 4 + gi * 8:12 + gi * 8], scalar1=pen[:, gi:gi + 1], scalar2=None, op0=ALU.add),
           reads=[K, "rt_e"], writes=["rt_fm"])
    op("dve", lambda e: e.max(out=top8, in_=fm), reads=["rt_fm"], writes=["rt_t8"])
    op("dve", lambda e: e.tensor_scalar(out=eq1, in0=fm, scalar1=top8[:, 0:1], scalar2=None, op0=ALU.is_equal), reads=["rt_fm", "rt_t8"], writes=["rt_eq1"])
    op("dve", lambda e: e.tensor_scalar(out=eq2, in0=fm, scalar1=top8[:, 1:2], scalar2=None, op0=ALU.is_equal), reads=["rt_fm", "rt_t8"], writes=["rt_eq2"])
    dd, ee = sm[:, 32:33], sm[:, 33:34]
    op("dve", lambda e: e.tensor_tensor(out=dd, in0=top8[:, 1:2], in1=top8[:, 0:1], op=ALU.subtract), reads=["rt_t8"], writes=["rt_dd"])
    op("act", lambda e: e.activation(out=ee, in_=dd, func=AF.Exp), reads=["rt_dd"], writes=["rt_ee"])
    op("dve", lambda e: e.tensor_scalar(out=ee, in0=ee, scalar1=1.0, scalar2=None, op0=ALU.add), reads=["rt_ee"], writes=["rt_ee"])
    op("dve", lambda e: e.reciprocal(out=ee, in_=ee), reads=["rt_ee"], writes=["rt_ee"])
    c1, c2 = 2 * t, 2 * t + 1
    op("dve", lambda e: e.tensor_tensor(out=w12[:, c1:c1 + 1], in0=ee, in1=gp, op=ALU.mult), reads=["rt_ee", "rt_d"], writes=["w12"])
    op("dve", lambda e: e.tensor_tensor(out=w12[:, c2:c2 + 1], in0=gp, in1=w12[:, c1:c1 + 1], op=ALU.subtract), reads=["rt_d", "w12"], writes=["w12"])
    op("dve", lambda e: e.tensor_tensor(out=oh, in0=eq1, in1=eq2, op=ALU.add), reads=["rt_eq1", "rt_eq2"], writes=["rt_oh"])
    if pre_pos_hook is not None:
        pre_pos_hook()
    op("pe", lambda e: e.matmul(pb[1][:, 0:32], ltri[:], oh, start=True, stop=True), reads=["ltri", "rt_oh"], writes=[PB[1]])
    op("pe", lambda e: e.matmul(pb[1][:, 64:96], ones_f[:], oh, start=True, stop=True, skip_group_check=True), reads=["ones_f", "rt_oh"], writes=[PB[1]])
    if mid_hook is not None:
        mid_hook()
    DUMMY = float(NE * CAP)
    op("dve", lambda e: e.tensor_tensor(out=tmp, in0=pb[1][:, 0:32], in1=rcb[:], op=ALU.add), reads=[PB[1], "rcb"], writes=["rt_tmp"])
    op("dve", lambda e: e.tensor_tensor(out=sl, in0=tmp, in1=sbase[:], op=ALU.add), reads=["rt_tmp", "sbase"], writes=["rt_sl"])
    op("dve", lambda e: e.tensor_scalar(out=tmp, in0=tmp, scalar1=float(CAP), scalar2=None, op0=ALU.is_lt), reads=["rt_tmp"], writes=["rt_tmp"])
    op("dve", lambda e: e.scalar_tensor_tensor(out=sl, in0=sl, scalar=-DUMMY, in1=tmp, op0=ALU.add, op1=ALU.mult), reads=["rt_sl", "rt_tmp"], writes=["rt_sl"])
    op("dve", lambda e: e.tensor_scalar(out=sl, in0=sl, scalar1=DUMMY, scalar2=None, op0=ALU.add), reads=["rt_sl"], writes=["rt_sl"])
    op("dve", lambda e: e.tensor_tensor(out=rcb[:], in0=rcb[:], in1=pb[1][:, 64:96], op=ALU.add), reads=[PB[1], "rcb"], writes=["rcb"])
    d1, d2 = sm[:, 34:35], sm[:, 35:36]
    op("dve", lambda e: e.tensor_tensor(out=tmp, in0=eq1, in1=sl, op=ALU.mult), reads=["rt_eq1", "rt_sl"], writes=["rt_tmp"])
    op("dve", lambda e: e.reduce_sum(out=d1, in_=tmp, axis=AX.X), reads=["rt_tmp"], writes=["rt_d1"])
    op("dve", lambda e: e.tensor_tensor(out=tmp, in0=eq2, in1=sl, op=ALU.mult), reads=["rt_eq2", "rt_sl", "rt_d1"], writes=["rt_tmp"])
    op("dve", lambda e: e.reduce_sum(out=d2, in_=tmp, axis=AX.X), reads=["rt_tmp"], writes=["rt_d2"])
    op("dve", lambda e: e.tensor_copy(out=dest_i[:, c1:c1 + 1], in_=d1), reads=["rt_d1"], writes=["dest_i"])
    op("dve", lambda e: e.tensor_copy(out=dest_i[:, c2:c2 + 1], in_=d2), reads=["rt_d2"], writes=["dest_i"])


_NC = None


def _prep_weights(inp):
    f = np.float32
    w_in = inp["w_in"][0]
    w = {}
    w["w_in_t"] = np.ascontiguousarray(w_in.reshape(16, 128, 24, 128).transpose(2, 1, 0, 3)).reshape(24, 128, 2048)
    w["w_out_t"] = np.ascontiguousarray(inp["w_out"][0].reshape(16, 128, D).transpose(1, 0, 2))
    w["w_kv_t"] = np.ascontiguousarray(inp["w_mem_kv"][0].reshape(16, 128, 1024).transpose(1, 0, 2))
    w["w_pool_t"] = np.ascontiguousarray(inp["w_pool"][0].transpose(1, 0, 2)).reshape(128, 512)
    w["w_a_t"] = np.ascontiguousarray(inp["w_a"][0].transpose(1, 0, 2)).reshape(128, 1024)
    w["w_x_t"] = np.ascontiguousarray(inp["w_x"][0].transpose(1, 0, 2)).reshape(128, 1024)
    w["lnp"] = np.stack([inp["ln1_g"][0], inp["ln1_b"][0], inp["ln2_g"][0], inp["ln2_b"][0]]).astype(f)
    wr = np.concatenate([inp["w_group"][0], inp["w_fine"][0]], axis=1)
    w["w_r_t"] = np.ascontiguousarray(wr.reshape(16, 128, 36).transpose(1, 0, 2)).reshape(128, 16 * 36)
    w["b_r"] = np.concatenate([inp["b_group"][0], inp["b_fine"][0].reshape(-1)]).reshape(1, 36).astype(f)
    w["wg_t"] = np.ascontiguousarray(inp["w_gate"][0].reshape(NE, 16, 128, 2, 512).transpose(0, 3, 2, 1, 4)).reshape(NE, 2, 128, 16 * 512)
    w["wu_t"] = np.ascontiguousarray(inp["w_up"][0].reshape(NE, 16, 128, 2, 512).transpose(0, 3, 2, 1, 4)).reshape(NE, 2, 128, 16 * 512)
    w["wd_t"] = np.ascontiguousarray(inp["w_down"][0].reshape(NE, 8, 128, 2, 1024).transpose(0, 3, 2, 1, 4)).reshape(NE, 2, 128, 8 * 1024)
    chv = np.zeros((128, NV), f)
    chv[:, O_PS:O_PS + 4] = inp["pool_scale"][0].reshape(4, 128).T
    cw = inp["conv_w"][0]
    chv[:, O_CW:O_CW + 32] = cw.reshape(4, 8, 128).transpose(2, 1, 0).reshape(128, 32)
    chv[:, O_CB:O_CB + 8] = inp["conv_b"][0].reshape(8, 128).T
    chv[:, O_BA:O_BA + 8] = inp["b_a"][0].reshape(8, 128).T
    chv[:, O_BX:O_BX + 8] = inp["b_x"][0].reshape(8, 128).T
    chv[:, O_LAM:O_LAM + 8] = inp["lam"][0].reshape(8, 128).T
    chv[:, O_G:O_G + 16] = inp["mix_norm_g"][0].reshape(16, 128).T
    return w, chv


def kernel(**inp):
    global _NC
    inp = {k: np.asarray(v) for k, v in inp.items()}
    x = inp["x"]
    mem = inp["mem"]
    w, chv0 = _prep_weights(inp)
    in_maps = []
    for c in range(8):
        b, q = c // 4, c % 4
        n = (q + 1) * T
        xe = np.zeros((NCH * CH, D), np.float32)
        xe[NCH * CH - n:] = x[b, :n]
        chv = chv0.copy()
        vm = np.zeros(16, np.float32)
        vm[16 - 4 * (q + 1):] = 1.0
        chv[:, O_VM:O_VM + 16] = vm[None, :]
        invc = np.zeros((128, 64), np.float32)
        for gi in range(4):
            wl = 2 ** (gi + 1)
            for tt in range(16):
                invc[:, gi * 16 + tt] = 1.0 / (min(tt + 1, wl) if q == 0 else wl)
        m = dict(w)
        m.update({"xe": xe, "mem": np.ascontiguousarray(mem[b]), "chv": chv, "invcnt": invc})
        in_maps.append(m)
    if _NC is None:
        _NC = build_nc()
    res = run_bass_kernel_spmd(_NC, in_maps, core_ids=list(range(8)))
    outs = [res.results[c]["out"] for c in range(8)]
    full = np.stack(outs).reshape(2, 4 * T, D).astype(np.float32)
    kernel.last_results = res.results
    return full
```

```python
import numpy as np
from contextlib import ExitStack
import concourse.bass as bass
import concourse.mybir as mybir
from concourse.bass_utils import run_bass_kernel_spmd

F32 = mybir.dt.float32
F32R = mybir.dt.float32r
BF16 = mybir.dt.bfloat16
I32 = mybir.dt.int32
AF = mybir.ActivationFunctionType
ALU = mybir.AluOpType
AX = mybir.AxisListType

D = 2048
T = 2048
NCH = 16
NPRE = 12
CH = 512
CAP = 384
NSB = CAP // 128
NE = 32
ALPHA = 2.0 ** 0.25
EPS = 1e-5
DEBUG = False

O_PS, O_CW, O_CB, O_BA, O_BX, O_LAM, O_G, O_VM = 0, 4, 36, 44, 52, 60, 68, 84
NV = 100


class Prog:
    def __init__(self, nc, stack):
        self.nc = nc
        self.stack = stack
        self.eng = {"pe": nc.tensor, "act": nc.scalar, "dve": nc.vector,
                    "pool": nc.gpsimd, "sp": nc.sync}
        self.sem = {}
        for e in ("pe", "act", "dve", "pool"):
            self.sem[e] = stack.enter_context(nc.semaphore("sem_" + e))
        self.cnt = {e: 0 for e in ("pe", "act", "dve", "pool")}
        self.dsem = {}
        self.dcnt = {}
        self.last_w = {}
        self.readers = {}
        self.waited = {e: {} for e in ("pe", "act", "dve", "pool", "sp")}
        self.n_inst = 0

    def _tok_sem(self, tok):
        return self.sem[tok[1]] if tok[0] == "e" else self.dsem[tok[1]]

    def _wait(self, eng, tok):
        kind, key, val = tok
        name = (kind, key)
        if self.waited[eng].get(name, 0) >= val:
            return
        if kind == "e" and key == eng and eng == "pe":
            return
        self.waited[eng][name] = val
        self.eng[eng].wait_ge(self._tok_sem(tok), val)

    def _deps(self, eng, reads, writes):
        deps = []
        for r in reads:
            t = self.last_w.get(r)
            if t is not None:
                deps.append(t)
        for w in writes:
            t = self.last_w.get(w)
            if t is not None and not (t[0] == "e" and t[1] == eng):
                deps.append(t)
            for t in self.readers.get(w, ()):
                if not (t[0] == "e" and t[1] == eng):
                    deps.append(t)
        return deps

    def _commit(self, tok, reads, writes):
        for r in reads:
            self.readers.setdefault(r, []).append(tok)
        for w in writes:
            self.last_w[w] = tok
            self.readers[w] = []

    def op(self, eng, fn, reads=(), writes=(), inc=True):
        for t in self._deps(eng, reads, writes):
            self._wait(eng, t)
        ins = fn(self.eng[eng])
        self.n_inst += 1
        if inc:
            self.cnt[eng] += 1
            ins.then_inc(self.sem[eng], 1)
            tok = ("e", eng, self.cnt[eng])
        else:
            tok = ("e", eng, self.cnt[eng] + 1)
        self._commit(tok, reads, writes)
        return ins

    def dma(self, q, fn, reads=(), writes=(), key=None):
        if key is None:
            key = writes[0]
        if key not in self.dsem:
            self.dsem[key] = self.stack.enter_context(self.nc.semaphore("d_" + str(key)))
            self.dcnt[key] = 0
        for t in self._deps(q, reads, writes):
            self._wait(q, t)
        ins = fn(self.eng[q])
        self.n_inst += 1
        self.dcnt[key] += 16
        ins.then_inc(self.dsem[key], 16)
        tok = ("d", key, self.dcnt[key])
        self._commit(tok, reads, writes)
        return ins

    def _all_tokens(self):
        toks = [("e", e, self.cnt[e]) for e in ("pe", "act", "dve", "pool") if self.cnt[e] > 0]
        toks += [("d", k, v) for k, v in self.dcnt.items() if v > 0]
        return toks

    def barrier(self):
        toks = self._all_tokens()
        for e in ("pe", "act", "dve", "pool", "sp"):
            for t in toks:
                self._wait(e, t)
        self.last_w.clear()
        self.readers.clear()

    def finish(self, eng):
        for t in self._all_tokens():
            self._wait(eng, t)


def build_nc():
    nc = bass.Bass("TRN2", target_bir_lowering=False)
    nc.dge_precook = False
    dram = lambda name, shape, dt, kind="ExternalInput": nc.dram_tensor(name, shape, dt, kind=kind)
    xe = dram("xe", [NCH * CH, D], F32)
    mem = dram("mem", [256, D], F32)
    w_in_t = dram("w_in_t", [24, 128, 16 * 128], F32)
    w_out_t = dram("w_out_t", [128, 16, D], F32)
    w_kv_t = dram("w_kv_t", [128, 16, 1024], F32)
    w_pool_t = dram("w_pool_t", [128, 4 * 128], F32)
    w_a_t = dram("w_a_t", [128, 8 * 128], F32)
    w_x_t = dram("w_x_t", [128, 8 * 128], F32)
    chv_d = dram("chv", [128, NV], F32)
    invc_d = dram("invcnt", [128, 64], F32)
    lnp = dram("lnp", [4, D], F32)
    w_r_t = dram("w_r_t", [128, 16 * 36], F32)
    b_r = dram("b_r", [1, 36], F32)
    wg_t = dram("wg_t", [NE, 2, 128, 16 * 512], F32R)
    wu_t = dram("wu_t", [NE, 2, 128, 16 * 512], F32R)
    wd_t = dram("wd_t", [NE, 2, 128, 8 * 1024], F32R)
    out = dram("out", [T, D], F32, kind="ExternalOutput")
    Xs = dram("Xs", [NE * CAP, D], F32, kind="Internal")
    Ys = dram("Ys", [NE * CAP + 1, D], F32, kind="Internal")
    X1s = dram("X1s", [T, D], F32, kind="Internal")
    if DEBUG:
        dbg_x1 = dram("dbg_x1", [T, D], F32, kind="ExternalOutput")
        dbg_rt = dram("dbg_rt", [T, 4], F32, kind="ExternalOutput")

    with ExitStack() as g:
        P = Prog(nc, g)

        def sbin(st, name, shape, dt):
            return st.enter_context(nc.sbuf_tensor(name, shape, dt))

        pb = [g.enter_context(nc.psum_tensor("pb%d" % i, [128, 512], F32)) for i in range(8)]
        PB = ["pb%d" % i for i in range(8)]

        ident = sbin(g, "ident", [128, 128], F32)
        ones_f = sbin(g, "ones_f", [128, 128], F32)
        ones_b = sbin(g, "ones_b", [128, 128], BF16)
        ltri = sbin(g, "ltri", [128, 128], F32)
        chv = sbin(g, "chv_s", [128, NV], F32)
        invc = sbin(g, "invc_s", [128, 64], F32)
        cl = sbin(g, "cl", [128, 32], F32)
        vmh = sbin(g, "vmh", [128, 16], F32)
        wpool_b = sbin(g, "wpool_b", [128, 4 * 128], BF16)
        wa_b = sbin(g, "wa_b", [128, 8 * 128], BF16)
        wx_b = sbin(g, "wx_b", [128, 8 * 128], BF16)
        kT = sbin(g, "kT", [128, 4, 256], BF16)
        vv = sbin(g, "vv", [128, 2, 512], BF16)
        hst = sbin(g, "hst", [128, 8], F32)
        w_r = sbin(g, "w_r", [128, 16, 36], F32)
        b_rb = sbin(g, "b_rb", [128, 36], F32)
        rcb = sbin(g, "rcb", [128, 32], F32)
        sbase = sbin(g, "sbase", [128, 32], F32)
        dest_i = sbin(g, "dest_i", [128, 32], I32)
        w12 = sbin(g, "w12", [128, 32], F32)
        sm = sbin(g, "sm", [128, 64], F32)
        sm32 = sbin(g, "sm32", [128, 8, 36], F32)

        dma = P.dma
        op = P.op

        op("pool", lambda e: e.memset(ident[:], 0.0), writes=["ident"])
        op("pool", lambda e: e.affine_select(out=ident[:], in_=ident[:], pattern=[[-1, 128]],
                                             compare_op=ALU.not_equal, fill=1.0, base=0, channel_multiplier=1),
           reads=["ident"], writes=["ident"])
        op("pool", lambda e: e.memset(ones_f[:], 1.0), writes=["ones_f"])
        op("pool", lambda e: e.memset(ones_b[:], 1.0), writes=["ones_b"])
        op("pool", lambda e: e.affine_select(out=ltri[:], in_=ones_f[:], pattern=[[1, 128]],
                                             compare_op=ALU.is_gt, fill=0.0, base=0, channel_multiplier=-1),
           reads=["ones_f"], writes=["ltri"])
        op("pool", lambda e: e.memset(hst[:], 0.0), writes=["hst"])
        op("pool", lambda e: e.iota(dest_i[:], pattern=[[CAP, 32]], base=0, channel_multiplier=0), writes=["dest_i"])
        op("dve", lambda e: e.tensor_copy(out=sbase[:], in_=dest_i[:]), reads=["dest_i"], writes=["sbase"])
        op("pool", lambda e: e.memset(rcb[:], 0.0), writes=["rcb"])
        dma("sp", lambda e: e.dma_start(out=chv[:], in_=chv_d.ap()), writes=["chv"])
        dma("sp", lambda e: e.dma_start(out=invc[:], in_=invc_d.ap()), writes=["invc"])
        dma("sp", lambda e: e.dma_start(out=w_r[:], in_=w_r_t.ap().rearrange("p (k n) -> p k n", n=36)), writes=["w_r"])
        dma("sp", lambda e: e.dma_start(out=b_rb[:], in_=b_r.ap().partition_broadcast(128)), writes=["b_rb"])
        op("act", lambda e: e.activation(out=sm[:, 0:8], in_=chv[:, O_LAM:O_LAM + 8], func=AF.Exp, scale=-1.0),
           reads=["chv"], writes=["sm"])
        op("act", lambda e: e.activation(out=sm[:, 8:16], in_=sm[:, 0:8], func=AF.Ln, bias=1.0),
           reads=["sm"], writes=["sm"])
        op("dve", lambda e: e.tensor_scalar_mul(out=cl[:, 0:8], in0=sm[:, 8:16], scalar1=-4.0), reads=["sm"], writes=["cl"])
        op("dve", lambda e: e.tensor_scalar_mul(out=cl[:, 8:16], in0=sm[:, 8:16], scalar1=-8.0), reads=["sm"], writes=["cl"])
        op("dve", lambda e: e.tensor_scalar_mul(out=cl[:, 16:24], in0=chv[:, O_BA:O_BA + 8], scalar1=0.5), reads=["chv"], writes=["cl"])
        op("dve", lambda e: e.tensor_scalar_mul(out=cl[:, 24:32], in0=chv[:, O_BX:O_BX + 8], scalar1=0.5), reads=["chv"], writes=["cl"])
        op("dve", lambda e: e.tensor_scalar_mul(out=vmh[:], in0=chv[:, O_VM:O_VM + 16], scalar1=0.5), reads=["chv"], writes=["vmh"])

        with ExitStack() as sa:
            stg = sbin(sa, "stgA", [128, 1024], F32)
            memx = sbin(sa, "memx", [128, 2, D], F32)
            memT = sbin(sa, "memT", [128, 16, 256], BF16)
            wkv_s = [sbin(sa, "wkv_s%d" % i, [128, 1024], F32) for i in range(4)]
            wkv_b = [sbin(sa, "wkv_b%d" % i, [128, 1024], BF16) for i in range(4)]
            for (src, dstb, n, nm) in ((w_pool_t, wpool_b, 512, "wpool_b"), (w_a_t, wa_b, 1024, "wa_b"), (w_x_t, wx_b, 1024, "wx_b")):
                dma("sp", lambda e, src=src, n=n: e.dma_start(out=stg[:, 0:n], in_=src.ap()), writes=["stgA"])
                op("dve", lambda e, dstb=dstb, n=n: e.tensor_copy(out=dstb[:], in_=stg[:, 0:n]), reads=["stgA"], writes=[nm])
            for mt in range(2):
                dma("sp", lambda e, mt=mt: e.dma_start(out=memx[:, mt, :], in_=mem[mt * 128:(mt + 1) * 128, :]), writes=["memx%d" % mt])
            for mt in range(2):
                for g4 in range(4):
                    bk = g4 % 2
                    for i4 in range(4):
                        kc = g4 * 4 + i4
                        op("pe", lambda e, mt=mt, kc=kc, i4=i4, bk=bk: e.transpose(pb[bk][:, i4 * 128:(i4 + 1) * 128], memx[:, mt, kc * 128:(kc + 1) * 128], ident[:]),
                           reads=["memx%d" % mt, "ident"], writes=[PB[bk]], inc=(i4 == 3))
                    op("act" if g4 % 2 == 0 else "dve",
                       (lambda e, mt=mt, g4=g4, bk=bk: e.activation(out=memT[:, g4 * 4:(g4 + 1) * 4, mt * 128:(mt + 1) * 128], in_=pb[bk][:].rearrange("p (a b) -> p a b", b=128), func=AF.Copy))
                       if g4 % 2 == 0 else
                       (lambda e, mt=mt, g4=g4, bk=bk: e.tensor_copy(out=memT[:, g4 * 4:(g4 + 1) * 4, mt * 128:(mt + 1) * 128], in_=pb[bk][:].rearrange("p (a b) -> p a b", b=128))),
                       reads=[PB[bk]], writes=["memT"])
            for kc in range(16):
                s = kc % 4
                dma("sp", lambda e, kc=kc, s=s: e.dma_start(out=wkv_s[s][:], in_=w_kv_t[:, kc, :]), writes=["wkv_s%d" % s])
                op("dve", lambda e, s=s: e.tensor_copy(out=wkv_b[s][:], in_=wkv_s[s][:]), reads=["wkv_s%d" % s], writes=["wkv_b%d" % s])
                for h in range(4):
                    bk = h
                    op("pe", lambda e, h=h, kc=kc, s=s, bk=bk: e.matmul(pb[bk][:, 0:256], wkv_b[s][:, h * 128:(h + 1) * 128], memT[:, kc, :],
                                                                        start=(kc == 0), stop=(kc == 15)),
                       reads=["wkv_b%d" % s, "memT"], writes=[PB[bk]], inc=False)
                for mc in range(2):
                    op("pe", lambda e, mc=mc, kc=kc, s=s: e.matmul(pb[4 + mc][:], memT[:, kc, mc * 128:(mc + 1) * 128], wkv_b[s][:, 512:1024],
                                                                   start=(kc == 0), stop=(kc == 15)),
                       reads=["wkv_b%d" % s, "memT"], writes=[PB[4 + mc]], inc=(mc == 1))
            for h in range(4):
                bk = h
                op("act", lambda e, h=h, bk=bk: e.activation(out=kT[:, h, :], in_=pb[bk][:, 0:256], func=AF.Copy),
                   reads=[PB[bk]], writes=["kT"])
            for mc in range(2):
                op("dve", lambda e, mc=mc: e.tensor_copy(out=vv[:, mc, :], in_=pb[4 + mc][:]), reads=[PB[4 + mc]], writes=["vv"])
            P.barrier()

        with ExitStack() as sbs:
            yT = sbin(sbs, "yT", [128, 16, T], BF16)
            sp1 = sbs.enter_context(ExitStack())
            uext = sbin(sp1, "uext", [128, 8, CH + 3], BF16)
            upx = sbin(sp1, "upx", [128, 4, CH + 16], F32)
            op("pool", lambda e: e.memset(uext[:], 0.0), writes=["uext%d" % i for i in range(8)])
            op("pool", lambda e: e.memset(upx[:], 0.0), writes=["upx%d" % i for i in range(4)])
            yT_box = [yT]
            dg = sbin(sp1, "dg", [128, 8, 4, 128], BF16)
            for blk_ in range(8):
                for k_ in range(4):
                    cwc = O_CW + blk_ * 4 + k_
                    op("dve" if (blk_ * 4 + k_) % 2 == 0 else "pool",
                       lambda e, blk_=blk_, k_=k_, cwc=cwc: e.tensor_scalar(out=dg[:, blk_, k_, :], in0=ident[:], scalar1=chv[:, cwc:cwc + 1], scalar2=None, op0=ALU.mult),
                       reads=["ident", "chv"], writes=["dg"])

            def phase1(chunks, NS, own_phase):
              with ExitStack() as s1:
                yT = yT_box[0]
                tag = "o_" if own_phase else "p_"
                xin = [sbin(s1, tag + "xin%d" % i, [128, D], F32) for i in range(2)]
                xT = [sbin(s1, tag + "xT%d" % i, [128, 16, CH], BF16) for i in range(2)]
                wbf = [sbin(s1, tag + "wbf%d" % i, [128, 16, 128], BF16) for i in range(3)]
                NW = 9
                A = []
                for si in range(2):
                    tl = [sbin(s1, tag + "A%d_%d" % (si, i), [128, CH], F32) for i in range(4)]
                    A.append([tl[0][:], tl[1][:], tl[2][:], tl[3][:], tl[1][:]])
                ucbs = sbin(s1, tag + "ucbs", [128, 2, CH], BF16)
                ucb_aps = [ucbs[:, 0, :], ucbs[:, 1, :]]
                if NS > 2:
                    def ht(i):
                        return yT[:, i // 2, :].bitcast(F32)[:, (i % 2) * CH:(i % 2 + 1) * CH]
                    for si in range(2, NS):
                        tl = [ht(4 * (si - 2) + j_) for j_ in range(4)]
                        A.append([tl[0], tl[1], tl[2], tl[3], tl[1]])
                        ucb_aps.append(yT[:, 12 + (si - 2) // 4, ((si - 2) % 4) * CH:((si - 2) % 4 + 1) * CH])

                def akeys(st):
                    return ["A%d_0" % st, "A%d_1" % st, "A%d_2" % st, "A%d_3" % st, "A%d_1" % st]

                if own_phase:
                    W = [None] * 6 + [sbin(s1, tag + "W%d" % i, [128, CH + 16], F32) for i in range(6, NW)]
                    Wb = [None, sbin(s1, tag + "Wb1", [128, 2, CH], BF16), sbin(s1, tag + "Wb2", [128, 1, CH], BF16)]
                    vg = sbin(s1, tag + "vg", [128, 3, CH], F32)

                items = []
                for ci in chunks:
                    own = ci >= NPRE
                    for pr in range(4):
                        for blk in (2 * pr, 2 * pr + 1):
                            items.append((ci, 4 + blk, "u", blk))
                        if own:
                            items.append((ci, pr, "p", pr))
                            for blk in (2 * pr, 2 * pr + 1):
                                items.append((ci, 12 + blk, "g", blk))
                            items.append((ci, 20 + pr, "m", pr))
                    if ci == NPRE - 1:
                        for gi in range(4):
                            items.append((ci, gi, "ph", gi))
                NI = len(items)

                def load_w(n):
                    ci, j, kind, idx = items[n]
                    s = n % 3
                    dma("pool", lambda e: e.dma_start(out=wbf[s][:].rearrange("p a b -> p (a b)"), in_=w_in_t[j], max_dma_last_dim=4096),
                        writes=["wbf%d" % s])

                def prep_chunk(ci):
                    b = ci % 2
                    for tt in range(4):
                        xi = tt % 2
                        r0 = ci * CH + tt * 128
                        dma("sp", lambda e: e.dma_start(out=xin[xi][:], in_=xe[r0:r0 + 128, :]), writes=["xin%d" % xi])
                        for g4 in range(4):
                            bk = 0
                            for i4 in range(4):
                                kc = g4 * 4 + i4
                                op("pe", lambda e, kc=kc, i4=i4: e.transpose(pb[bk][:, i4 * 128:(i4 + 1) * 128], xin[xi][:, kc * 128:(kc + 1) * 128], ident[:]),
                                   reads=["xin%d" % xi, "ident"], writes=[PB[bk]], inc=(i4 == 3))
                            dst = xT[b][:, g4 * 4:(g4 + 1) * 4, tt * 128:(tt + 1) * 128]
                            src = pb[bk][:].rearrange("p (a b) -> p a b", b=128)
                            if g4 % 2 == 0:
                                op("act", lambda e: e.activation(out=dst, in_=src, func=AF.Copy), reads=[PB[bk]], writes=["xT%d" % b])
                            else:
                                op("dve", lambda e: e.tensor_copy(out=dst, in_=src), reads=[PB[bk]], writes=["xT%d" % b])

                def inproj(n, bank, ncols=CH, c0=0):
                    ci, j, kind, idx = items[n]
                    s = n % 3
                    b = ci % 2
                    for kc in range(16):
                        op("pe", lambda e, kc=kc: e.matmul(pb[bank][:, 0:ncols], wbf[s][:, kc, :], xT[b][:, kc, c0:c0 + ncols],
                                                          start=(kc == 0), stop=(kc == 15)),
                           reads=["wbf%d" % s, "xT%d" % b], writes=[PB[bank]], inc=(kc == 15))

                def ssq_accum(ytile_key, ysrc, first, last, gidx=1):
                    if gidx == 1:
                        op("act", lambda e: e.activation(out=Wb[2][:, 0, :], in_=ysrc, func=AF.Square), reads=[ytile_key], writes=["Wb2_0"])
                        op("pe", lambda e: e.matmul(pb[6][:], ones_b[:], Wb[2][:, 0, :], start=first, stop=last),
                           reads=["Wb2_0", "ones_b"], writes=[PB[6]], inc=True)
                        return
                    vk = "vg%d" % gidx
                    if first:
                        op("act", lambda e: e.activation(out=vg[:, gidx, :], in_=ysrc, func=AF.Square), reads=[ytile_key], writes=[vk])
                    else:
                        op("act", lambda e: e.activation(out=W[6][:, 0:CH], in_=ysrc, func=AF.Square), reads=[ytile_key], writes=["W6"])
                        op("pool", lambda e: e.tensor_tensor(out=vg[:, gidx, :], in0=vg[:, gidx, :], in1=W[6][:, 0:CH], op=ALU.add),
                           reads=[vk, "W6"], writes=[vk])
                    if last:
                        op("pe", lambda e: e.matmul(pb[4][:], ones_f[:], vg[:, gidx, :], start=True, stop=True),
                           reads=["ones_f", vk], writes=[PB[4]])

                def group_var(gidx, n_ch):
                    bank = 6 if gidx == 1 else 4
                    op("dve", lambda e: e.tensor_scalar(out=vg[:, gidx, :], in0=pb[bank][:], scalar1=1.0 / n_ch, scalar2=EPS, op0=ALU.mult, op1=ALU.add),
                       reads=[PB[bank]], writes=["vg%d" % gidx])

                setof = {}

                def ubank(n):
                    return (2, 3, 6)[n % 3] if not own_phase else 2 + (n % 2)

                def lru_s1(n):
                    lru_s1a(n)
                    lru_s1b(n)

                def lru_s1a(n):
                    ci, j, kind, blk = items[n]
                    bank = ubank(n)
                    inproj(n, bank)
                    ue = "uext%d" % blk
                    op("dve", lambda e: e.tensor_copy(out=uext[:, blk, 0:3], in_=uext[:, blk, CH:CH + 3]), reads=[ue], writes=[ue])
                    op("dve", lambda e: e.tensor_copy(out=uext[:, blk, 3:CH + 3], in_=pb[bank][:]), reads=[PB[bank]], writes=[ue])

                def lru_s1b(n):
                    ci, j, kind, blk = items[n]
                    st = setof[n]
                    ucb = ucb_aps[st]
                    ucbk = "ucb%d" % st
                    bank = ubank(n)
                    ue = "uext%d" % blk
                    for k in range(4):
                        op("pe", lambda e, k=k: e.matmul(pb[bank][:], dg[:, blk, k, :], uext[:, blk, k:k + CH], start=(k == 0), stop=(k == 3)),
                           reads=["dg", ue], writes=[PB[bank]], inc=(k == 3))
                    op("act", lambda e: e.activation(out=ucb, in_=pb[bank][:], func=AF.Identity, bias=chv[:, O_CB + blk:O_CB + blk + 1]),
                       reads=[PB[bank], "chv"], writes=[ucbk])

                def lru_s2(n):
                    ci, j, kind, blk = items[n]
                    st = setof[n]
                    AK = akeys(st)
                    acc, thr, thi, aa, a2 = A[st]
                    ucb = ucb_aps[st]
                    ucbk = "ucb%d" % st
                    pr_, pi_ = (4, 5) if st % 2 == 0 else (1, 7)
                    op("pe", lambda e: e.matmul(pb[pr_][:], wa_b[:, blk * 128:(blk + 1) * 128], ucb, start=True, stop=True),
                       reads=["wa_b", ucbk], writes=[PB[pr_]])
                    op("pe", lambda e: e.matmul(pb[pi_][:], wx_b[:, blk * 128:(blk + 1) * 128], ucb, start=True, stop=True),
                       reads=["wx_b", ucbk], writes=[PB[pi_]])
                    op("act", lambda e: e.activation(out=thr[:], in_=pb[pr_][:], func=AF.Tanh, scale=0.5, bias=cl[:, 16 + blk:17 + blk]),
                       reads=[PB[pr_], "cl"], writes=[AK[1]])
                    op("act", lambda e: e.activation(out=thi[:], in_=pb[pi_][:], func=AF.Tanh, scale=0.5, bias=cl[:, 24 + blk:25 + blk]),
                       reads=[PB[pi_], "cl"], writes=[AK[2]])
                    op("act", lambda e: e.activation(out=aa[:], in_=thr[:], func=AF.Exp, scale=cl[:, blk:blk + 1], bias=cl[:, blk:blk + 1]),
                       reads=[AK[1], "cl"], writes=[AK[3]])
                    op("act", lambda e: e.activation(out=a2[:], in_=thr[:], func=AF.Exp, scale=cl[:, 8 + blk:9 + blk], bias=cl[:, 8 + blk:9 + blk]),
                       reads=[AK[1], "cl"], writes=[AK[4]])

                def lru_s3(n):
                    ci, j, kind, blk = items[n]
                    st = setof[n]
                    AK = akeys(st)
                    acc, thr, thi, aa, a2 = A[st]
                    ucb = ucb_aps[st]
                    ucbk = "ucb%d" % st
                    op("dve", lambda e: e.tensor_scalar_min(out=a2[:], in0=a2[:], scalar1=0.9999999), reads=[AK[4]], writes=[AK[4]])
                    op("dve", lambda e: e.scalar_tensor_tensor(out=thi[:], in0=thi[:], scalar=1.0, in1=ucb, op0=ALU.add, op1=ALU.mult),
                       reads=[AK[2], ucbk], writes=[AK[2]])

                def lru_s4a(n):
                    ci, j, kind, blk = items[n]
                    st = setof[n]
                    AK = akeys(st)
                    acc, thr, thi, aa, a2 = A[st]
                    op("act", lambda e: e.activation(out=a2[:], in_=a2[:], func=AF.Sqrt, scale=-1.0, bias=1.0), reads=[AK[4]], writes=[AK[4]])

                def lru_s4b(n):
                    ci, j, kind, blk = items[n]
                    st = setof[n]
                    AK = akeys(st)
                    acc, thr, thi, aa, a2 = A[st]
                    op("dve", lambda e: e.scalar_tensor_tensor(out=a2[:], in0=a2[:], scalar=vmh[:, ci:ci + 1], in1=thi[:], op0=ALU.mult, op1=ALU.mult),
                       reads=[AK[4], AK[2], "vmh"], writes=[AK[4]])
                    op("dve", lambda e: e.tensor_tensor_scan(out=acc[:], data0=aa[:], data1=a2[:], initial=hst[:, blk:blk + 1],
                                                             op0=ALU.mult, op1=ALU.add),
                       reads=[AK[3], AK[4], "hst"], writes=[AK[0]])
                    op("pool", lambda e: e.tensor_copy(out=hst[:, blk:blk + 1], in_=acc[:, CH - 1:CH]), reads=[AK[0]], writes=["hst"])

                def lru_g(n, do_inproj=True):
                    ci, j, kind, blk = items[n]
                    st = blk % 2
                    hk = "A%d_0" % st
                    oc = ci - NPRE
                    bank = 2 + (n % 2)
                    if do_inproj:
                        inproj(n, bank)
                    gg = W[6]
                    op("act", lambda e: e.activation(out=gg[:, 0:CH], in_=pb[bank][:], func=AF.Gelu_apprx_tanh), reads=[PB[bank]], writes=["W6"])
                    ysl = yT[:, 4 + blk, oc * CH:(oc + 1) * CH]
                    yk = "yT%d" % (4 + blk)
                    op("dve", lambda e: e.tensor_tensor(out=ysl, in0=gg[:, 0:CH], in1=A[st][0][:], op=ALU.mult), reads=["W6", hk], writes=[yk])
                    ssq_accum(yk, ysl, blk == 0, blk == 7)
                    if blk == 7:
                        group_var(1, 1024.0)

                def pool_blk(n, halo_only=False, do_inproj=True):
                    ci, j, kind, gi = items[n]
                    bank = 2 + (n % 2)
                    pk = "upx%d" % gi
                    if halo_only:
                        inproj(n, bank, ncols=16, c0=CH - 16)
                        op("act", lambda e: e.activation(out=upx[:, gi, CH:CH + 16], in_=pb[bank][:, 0:16], func=AF.Copy), reads=[PB[bank]], writes=[pk])
                        return
                    oc = ci - NPRE
                    if do_inproj:
                        inproj(n, bank)
                    op("dve", lambda e: e.tensor_copy(out=upx[:, gi, 0:16], in_=upx[:, gi, CH:CH + 16]), reads=[pk], writes=[pk])
                    op("act", lambda e: e.activation(out=upx[:, gi, 16:CH + 16], in_=pb[bank][:], func=AF.Copy), reads=[PB[bank]], writes=[pk])
                    src = upx[:, gi, :]
                    srck = pk
                    step = 1
                    lo = 0
                    for lvl in range(gi + 1):
                        dstt = W[7 + (lvl % 2)]
                        dk = "W%d" % (7 + (lvl % 2))
                        lo2 = lo + step
                        op("pool", lambda e, src=src, dstt=dstt, lo2=lo2, step=step: e.tensor_tensor(out=dstt[:, lo2:CH + 16], in0=src[:, lo2:CH + 16], in1=src[:, lo2 - step:CH + 16 - step], op=ALU.add),
                           reads=[srck], writes=[dk])
                        src, srck, lo, step = dstt, dk, lo2, step * 2
                    wlen = float(2 ** (gi + 1))
                    dbf = Wb[1][:, 0, :]
                    op("dve", lambda e: e.scalar_tensor_tensor(out=dbf, in0=src[:, 16:CH + 16], scalar=1.0 / wlen, in1=upx[:, gi, 16:CH + 16], op0=ALU.mult, op1=ALU.subtract),
                       reads=[srck, pk], writes=["Wb1_0"])
                    if oc == 0:
                        tmp = W[6]
                        op("dve", lambda e: e.tensor_tensor(out=tmp[:, 0:16], in0=src[:, 16:32], in1=invc[:, gi * 16:(gi + 1) * 16], op=ALU.mult),
                           reads=[srck, "invc"], writes=["W6"])
                        op("dve", lambda e: e.tensor_tensor(out=Wb[1][:, 0, 0:16], in0=tmp[:, 0:16], in1=upx[:, gi, 16:32], op=ALU.subtract),
                           reads=["W6", pk, "Wb1_0"], writes=["Wb1_0"])
                    op("pe", lambda e: e.matmul(pb[4][:], wpool_b[:, gi * 128:(gi + 1) * 128], dbf, start=True, stop=True),
                       reads=["wpool_b", "Wb1_0"], writes=[PB[4]])
                    ysl = yT[:, gi, oc * CH:(oc + 1) * CH]
                    yk = "yT%d" % gi
                    op("act", lambda e: e.activation(out=ysl, in_=pb[4][:], func=AF.Identity, scale=chv[:, O_PS + gi:O_PS + gi + 1]),
                       reads=[PB[4], "chv"], writes=[yk])
                    ssq_accum(yk, ysl, gi == 0, gi == 3, gidx=0)
                    if gi == 3:
                        group_var(0, 512.0)

                def mem_blk(n, do_inproj=True):
                    ci, j, kind, h = items[n]
                    oc = ci - NPRE
                    bank = 2 + (n % 2)
                    if do_inproj:
                        inproj(n, bank)
                    qT = Wb[1][:, 1, :]
                    op("act", lambda e: e.activation(out=qT, in_=pb[bank][:], func=AF.Copy, scale=128.0 ** -0.5), reads=[PB[bank]], writes=["Wb1_1"])
                    for mc in range(2):
                        op("pe", lambda e, mc=mc: e.matmul(pb[4 + mc][:], kT[:, h, mc * 128:(mc + 1) * 128], qT, start=True, stop=True),
                           reads=["kT", "Wb1_1"], writes=[PB[4 + mc]])
                    for mc in range(2):
                        op("act", lambda e, mc=mc: e.activation(out=ucbs[:, mc, :], in_=pb[4 + mc][:], func=AF.Exp), reads=[PB[4 + mc]], writes=["ucb%d" % mc])
                    for mc in range(2):
                        op("pe", lambda e, mc=mc: e.matmul(pb[4][:], vv[:, mc, h * 128:(h + 1) * 128], ucbs[:, mc, :], start=(mc == 0), stop=(mc == 1)),
                           reads=["vv", "ucb%d" % mc], writes=[PB[4]], inc=(mc == 1))
                    for mc in range(2):
                        op("pe", lambda e, mc=mc: e.matmul(pb[5][:], ones_b[:], ucbs[:, mc, :], start=(mc == 0), stop=(mc == 1)),
                           reads=["ones_b", "ucb%d" % mc], writes=[PB[5]], inc=(mc == 1))
                    rinv = W[6]
                    op("dve", lambda e: e.reciprocal(out=rinv[:, 0:CH], in_=pb[5][:]), reads=[PB[5]], writes=["W6"])
                    ysl = yT[:, 12 + h, oc * CH:(oc + 1) * CH]
                    yk = "yT%d" % (12 + h)
                    op("dve", lambda e: e.tensor_tensor(out=ysl, in0=pb[4][:], in1=rinv[:, 0:CH], op=ALU.mult), reads=[PB[4], "W6"], writes=[yk])
                    ssq_accum(yk, ysl, h == 0, h == 3, gidx=2)
                    if h == 3:
                        group_var(2, 512.0)

                def finish_chunk(ci):
                    oc = ci - NPRE
                    for gidx in range(3):
                        op("act", lambda e, gidx=gidx: e.activation(out=vg[:, gidx, :], in_=vg[:, gidx, :], func=AF.Sqrt), reads=["vg%d" % gidx], writes=["vg%d" % gidx])
                    for gidx in range(3):
                        op("dve", lambda e, gidx=gidx: e.reciprocal(out=vg[:, gidx, :], in_=vg[:, gidx, :]), reads=["vg%d" % gidx], writes=["vg%d" % gidx])
                    for blk16 in range(16):
                        gidx = 0 if blk16 < 4 else (1 if blk16 < 12 else 2)
                        ysl = yT[:, blk16, oc * CH:(oc + 1) * CH]
                        eng = "dve" if blk16 % 2 == 0 else "pool"
                        op(eng, lambda e, ysl=ysl, gidx=gidx: e.tensor_tensor(out=ysl, in0=ysl, in1=vg[:, gidx, :], op=ALU.mult),
                           reads=["yT%d" % blk16, "vg%d" % gidx], writes=["yT%d" % blk16])

                def prefetch(n):
                    if n + 2 < NI:
                        load_w(n + 2)

                prep_chunk(chunks[0])
                load_w(0)
                if NI > 1:
                    load_w(1)
                if not own_phase:
                    GS = NS // 2
                    us = [n for n in range(NI) if items[n][2] == "u"]
                    rest = [n for n in range(NI) if items[n][2] != "u"]
                    upairs = [tuple(us[i:i + GS]) for i in range(0, len(us), GS)]
                    for g_, grp in enumerate(upairs):
                        for ii, n in enumerate(grp):
                            setof[n] = (g_ % 2) * GS + ii
                    G = len(upairs)
                    for g_ in range(G + 1):
                        prev = upairs[g_ - 1] if g_ >= 1 else None
                        cur = upairs[g_] if g_ < G else None
                        for i in range(GS + 1):
                            if cur and i < GS:
                                prefetch(cur[i])
                                lru_s1a(cur[i])
                                ci, j, kind, idx = items[cur[i]]
                                if i == 0 and idx == 0 and ci + 1 <= chunks[-1]:
                                    prep_chunk(ci + 1)
                            if cur and i >= 1:
                                lru_s1b(cur[i - 1])
                            if prev and i >= 1:
                                lru_s2(prev[i - 1])
                                lru_s3(prev[i - 1])
                        if prev:
                            for n in prev:
                                lru_s4a(n)
                            for n in prev:
                                lru_s4b(n)
                    for n in rest:
                        prefetch(n)
                        pool_blk(n, halo_only=True)
                else:
                    def ip(m):
                        prefetch(m)
                        inproj(m, 2 + (m % 2))

                    n = 0
                    while n < NI:
                        ci, j, kind, idx = items[n]
                        na, nb, np_, nga, ngb, nm = n, n + 1, n + 2, n + 3, n + 4, n + 5
                        setof[na], setof[nb] = 0, 1
                        for m in (na, nb):
                            prefetch(m)
                            lru_s1a(m)
                        if idx == 0 and ci + 1 <= chunks[-1]:
                            prep_chunk(ci + 1)
                        for m in (na, nb):
                            lru_s1b(m)
                        ip(np_)
                        for m in (na, nb):
                            lru_s2(m)
                        ip(nga)
                        pool_blk(np_, do_inproj=False)
                        for m in (na, nb):
                            lru_s3(m)
                        for m in (na, nb):
                            lru_s4a(m)
                        for m in (na, nb):
                            lru_s4b(m)
                        lru_g(nga, do_inproj=False)
                        ip(ngb)
                        ip(nm)
                        lru_g(ngb, do_inproj=False)
                        mem_blk(nm, do_inproj=False)
                        last_of_chunk = (nm + 1 == NI) or (items[nm + 1][0] != ci)
                        if last_of_chunk:
                            finish_chunk(ci)
                        n += 6
                P.barrier()

            phase1(list(range(0, NPRE)), 8, False)
            phase1(list(range(NPRE, NCH)), 2, True)
            sp1.close()

            with ExitStack() as s2:
                woutg = sbin(s2, "woutg", [128, 16, D], BF16)
                with ExitStack() as s2a:
                    wos = [sbin(s2a, "wos%d" % i, [128, D], F32) for i in range(6)]
                    for kb in range(16):
                        s = kb % 6
                        dma("sp", lambda e, kb=kb, s=s: e.dma_start(out=wos[s][:], in_=w_out_t[:, kb, :]), writes=["wos%d" % s])
                        op("dve",
                           lambda e, kb=kb, s=s: e.tensor_scalar(out=woutg[:, kb, :], in0=wos[s][:], scalar1=chv[:, O_G + kb:O_G + kb + 1], scalar2=None, op0=ALU.mult),
                           reads=["wos%d" % s, "chv"], writes=["woutg"])
                    P.barrier()
                xin2 = [sbin(s2, "xin2_%d" % i, [128, D], F32) for i in range(2)]
                Z = [sbin(s2, "Z%d" % i, [128, D], F32) for i in range(2)]
                x1T = sbin(s2, "x1T", [128, 16, 128], F32)
                lng = sbin(s2, "lng", [128, D], F32)
                lnb = sbin(s2, "lnb", [128, D], F32)
                stats2 = [sbin(s2, "stats_%d" % i, [128, 4, 6], F32) for i in range(2)]
                dma("sp", lambda e: e.dma_start(out=lng[:], in_=lnp[0:1, :].partition_broadcast(128)), writes=["lng"])
                dma("sp", lambda e: e.dma_start(out=lnb[:], in_=lnp[1:2, :].partition_broadcast(128)), writes=["lnb"])
                def p2_load(t):
                    zi = t % 2
                    r0 = NPRE * CH + t * 128
                    dma("sp", lambda e: e.dma_start(out=xin2[zi][:], in_=xe[r0:r0 + 128, :]), writes=["xin2_%d" % zi])

                def p2_mm(t, cs):
                    for c in cs:
                        bank = 2 + (c % 2)
                        for kb in range(16):
                            op("pe", lambda e, kb=kb, c=c, bank=bank: e.matmul(pb[bank][:], yT[:, kb, t * 128:(t + 1) * 128], woutg[:, kb, c * 512:(c + 1) * 512],
                                                                              start=(kb == 0), stop=(kb == 15)),
                               reads=["yT", "woutg"], writes=[PB[bank]], inc=(kb == 15))

                def p2_evac(t, cs):
                    zi = t % 2
                    zk = "Z%d" % zi
                    xk = "xin2_%d" % zi
                    for c in cs:
                        bank = 2 + (c % 2)
                        op("dve", lambda e, c=c, bank=bank: e.scalar_tensor_tensor(out=Z[zi][:, c * 512:(c + 1) * 512], in0=xin2[zi][:, c * 512:(c + 1) * 512], scalar=ALPHA,
                                                                                   in1=pb[bank][:], op0=ALU.mult, op1=ALU.add),
                           reads=[xk, PB[bank]], writes=[zk])
                        op("dve", lambda e, c=c: e.bn_stats(out=stats2[zi][:, c, :], in_=Z[zi][:, c * 512:(c + 1) * 512]), reads=[zk], writes=["stats_%d" % zi])

                def p2_back(t):
                    zi = t % 2
                    zk = "Z%d" % zi
                    nxt = t + 1 if t + 1 < 16 else None
                    layer_norm_tail(P, zk, Z[zi], stats2[zi], sm, lng, lnb, "lng", "lnb", stats_key="stats_%d" % zi)
                    if nxt is not None:
                        p2_load(nxt)
                    for g4 in range(4):
                        bk = 4 + g4
                        for i4 in range(4):
                            kc = g4 * 4 + i4
                            op("pe", lambda e, kc=kc, i4=i4, bk=bk: e.transpose(pb[bk][:, i4 * 128:(i4 + 1) * 128], Z[zi][:, kc * 128:(kc + 1) * 128], ident[:]),
                               reads=[zk, "ident"], writes=[PB[bk]], inc=(i4 == 3))
                        op("act", lambda e, g4=g4, bk=bk: e.activation(out=x1T[:, g4 * 4:(g4 + 1) * 4, :], in_=pb[bk][:].rearrange("p (a b) -> p a b", b=128), func=AF.Copy),
                           reads=[PB[bk]], writes=["x1T"])
                    for kc in range(16):
                        op("pe", lambda e, kc=kc: e.matmul(pb[0][:, 0:36], x1T[:, kc, :], w_r[:, kc, :], start=(kc == 0), stop=(kc == 15)),
                           reads=["x1T", "w_r"], writes=[PB[0]], inc=(kc == 15))
                    route_tile(P, t, pb, PB, sm, sm32, b_rb, rcb, sbase, ltri, ones_f, dest_i, w12,
                               pre_pos_hook=(lambda: p2_mm(nxt, (0, 1))) if nxt is not None else None,
                               mid_hook=(lambda: p2_evac(nxt, (0, 1))) if nxt is not None else None)
                    if nxt is not None:
                        p2_mm(nxt, (2, 3))
                        p2_evac(nxt, (2, 3))
                    for k in range(2):
                        col = 2 * t + k
                        dma("pool", lambda e, col=col: e.indirect_dma_start(out=Xs[:, :], out_offset=bass.IndirectOffsetOnAxis(ap=dest_i[:, col:col + 1], axis=0),
                                                                            in_=Z[zi][:, :], in_offset=None, bounds_check=NE * CAP - 1, oob_is_err=False),
                            reads=[zk, "dest_i"], writes=["Xs"])
                    dma("pool", lambda e: e.dma_start(out=X1s[t * 128:(t + 1) * 128, :], in_=Z[zi][:]), reads=[zk], writes=["X1s"])
                    if DEBUG:
                        dma("sp", lambda e: e.dma_start(out=dbg_x1[t * 128:(t + 1) * 128, :], in_=Z[zi][:]), reads=[zk], writes=["dbg_x1"])
                p2_load(0)
                p2_mm(0, (0, 1))
                p2_evac(0, (0, 1))
                p2_mm(0, (2, 3))
                p2_evac(0, (2, 3))
                for t in range(16):
                    p2_back(t)
                P.barrier()

        with ExitStack() as s3:
            ring = [sbin(s3, "ring%d" % i, [128, 16 * 512], F32R) for i in range(3)]
            xs = [sbin(s3, "xs%d" % i, [128, D], F32) for i in range(2)]
            XT = [sbin(s3, "XT%d" % i, [128, 16, CAP], F32R) for i in range(2)]
            hT = sbin(s3, "hT", [128, 8, CAP], F32R)
            sg = [sbin(s3, "sg%d" % i, [128, CAP], F32) for i in range(2)]
            yo = [sbin(s3, "yo%d" % i, [128, 1024], F32) for i in range(3)]
            op("pool", lambda e: e.memset(sg[0][0:1, :], 0.0), writes=["sg0"])
            for cz in range(D // CAP + 1):
                w_ = min(CAP, D - cz * CAP)
                if w_ <= 0:
                    break
                dma("pool", lambda e, cz=cz, w_=w_: e.dma_start(out=Ys[NE * CAP:NE * CAP + 1, cz * CAP:cz * CAP + w_], in_=sg[0][0:1, 0:w_]),
                    reads=["sg0"], writes=["Ys"])
            pieces = []
            for e_ in range(NE):
                for n_ in range(2):
                    pieces.append((e_, "g", n_))
                    pieces.append((e_, "u", n_))
                for h_ in range(2):
                    pieces.append((e_, "d", h_))
            NP_ = len(pieces)
            yo_ctr = [0]

            def load_piece(i):
                e_, kind, n_ = pieces[i]
                r = i % 3
                src = {"g": wg_t, "u": wu_t, "d": wd_t}[kind]
                dma("sp", lambda e: e.dma_start(out=ring[r][:], in_=src[e_, n_]), writes=["ring%d" % r])

            def prep_expert(e_):
                b = e_ % 2
                for s in range(NSB):
                    xi = s % 2
                    r0 = e_ * CAP + s * 128
                    dma("sp", lambda e, xi=xi, r0=r0: e.dma_start(out=xs[xi][:], in_=Xs[r0:r0 + 128, :]), reads=["Xs"], writes=["xs%d" % xi])
                    for g4 in range(4):
                        bk = g4 % 2
                        for i4 in range(4):
                            kc = g4 * 4 + i4
                            op("pe", lambda e, xi=xi, kc=kc, i4=i4, bk=bk: e.transpose(pb[bk][:, i4 * 128:(i4 + 1) * 128], xs[xi][:, kc * 128:(kc + 1) * 128], ident[:]),
                               reads=["xs%d" % xi, "ident"], writes=[PB[bk]], inc=(i4 == 3))
                        op("act" if g4 % 2 == 0 else "dve",
                           (lambda e, s=s, g4=g4, bk=bk: e.activation(out=XT[b][:, g4 * 4:(g4 + 1) * 4, s * 128:(s + 1) * 128], in_=pb[bk][:].rearrange("p (a b) -> p a b", b=128), func=AF.Copy))
                           if g4 % 2 == 0 else
                           (lambda e, s=s, g4=g4, bk=bk: e.tensor_copy(out=XT[b][:, g4 * 4:(g4 + 1) * 4, s * 128:(s + 1) * 128], in_=pb[bk][:].rearrange("p (a b) -> p a b", b=128))),
                           reads=[PB[bk]], writes=["XT%d" % b])

            load_piece(0)
            load_piece(1)
            prep_expert(0)
            for i in range(NP_):
                e_, kind, n_ = pieces[i]
                b = e_ % 2
                r = i % 3
                if kind == "g":
                    if i + 2 < NP_:
                        load_piece(i + 2)
                    continue
                if kind == "u":
                    rg = (i - 1) % 3
                    for f in range(4):
                        fc = n_ * 4 + f
                        bg = 2 + (fc % 2)
                        bu = 4 + (fc % 2)
                        for kc in range(16):
                            op("pe", lambda e, kc=kc, f=f, bg=bg, rg=rg: e.matmul(pb[bg][:, 0:CAP], ring[rg][:, kc * 512 + f * 128:kc * 512 + (f + 1) * 128], XT[b][:, kc, :],
                                                                                  start=(kc == 0), stop=(kc == 15)),
                               reads=["ring%d" % rg, "XT%d" % b], writes=[PB[bg]], inc=(kc == 15))
                        for kc in range(16):
                            op("pe", lambda e, kc=kc, f=f, bu=bu: e.matmul(pb[bu][:, 0:CAP], ring[r][:, kc * 512 + f * 128:kc * 512 + (f + 1) * 128], XT[b][:, kc, :],
                                                                          start=(kc == 0), stop=(kc == 15)),
                               reads=["ring%d" % r, "XT%d" % b], writes=[PB[bu]], inc=(kc == 15))
                        si = fc % 2
                        op("act", lambda e, bg=bg, si=si: e.activation(out=sg[si][:], in_=pb[bg][:, 0:CAP], func=AF.Silu), reads=[PB[bg]], writes=["sg%d" % si])
                        op("dve", lambda e, bu=bu, si=si, fc=fc: e.tensor_tensor(out=hT[:, fc, :], in0=sg[si][:], in1=pb[bu][:, 0:CAP], op=ALU.mult),
                           reads=["sg%d" % si, PB[bu]], writes=["hT"])
                    if n_ == 1 and e_ + 1 < NE:
                        prep_expert(e_ + 1)
                else:
                    for s in range(NSB):
                        yi = yo_ctr[0] % 3
                        yo_ctr[0] += 1
                        for c in range(2):
                            bank = 6 + c
                            for fc in range(8):
                                op("pe", lambda e, fc=fc, s=s, c=c, bank=bank: e.matmul(pb[bank][:], hT[:, fc, s * 128:(s + 1) * 128], ring[r][:, fc * 1024 + c * 512:fc * 1024 + (c + 1) * 512],
                                                                                      start=(fc == 0), stop=(fc == 7)),
                                   reads=["hT", "ring%d" % r], writes=[PB[bank]], inc=(fc == 7))
                            if c == 0:
                                op("act", lambda e, yi=yi, bank=bank, c=c: e.activation(out=yo[yi][:, c * 512:(c + 1) * 512], in_=pb[bank][:], func=AF.Copy),
                                   reads=[PB[bank]], writes=["yo%d" % yi])
                            else:
                                op("dve", lambda e, yi=yi, bank=bank, c=c: e.tensor_copy(out=yo[yi][:, c * 512:(c + 1) * 512], in_=pb[bank][:]),
                                   reads=[PB[bank]], writes=["yo%d" % yi])
                        r0 = e_ * CAP + s * 128
                        dma("pool", lambda e, yi=yi, r0=r0, n_=n_: e.dma_start(out=Ys[r0:r0 + 128, n_ * 1024:(n_ + 1) * 1024], in_=yo[yi][:]),
                            reads=["yo%d" % yi], writes=["Ys"])
                if i + 2 < NP_:
                    load_piece(i + 2)
            P.barrier()

        with ExitStack() as s4:
            y1 = [sbin(s4, "y1_%d" % i, [128, D], F32) for i in range(3)]
            y2 = [sbin(s4, "y2_%d" % i, [128, D], F32) for i in range(3)]
            xr = [sbin(s4, "xr_%d" % i, [128, D], F32) for i in range(3)]
            lng = sbin(s4, "lng2", [128, D], F32)
            lnb = sbin(s4, "lnb2", [128, D], F32)
            stats3 = [sbin(s4, "stats3_%d" % i, [128, 4, 6], F32) for i in range(3)]
            dma("sp", lambda e: e.dma_start(out=lng[:], in_=lnp[2:3, :].partition_broadcast(128)), writes=["lng2"])
            dma("sp", lambda e: e.dma_start(out=lnb[:], in_=lnp[3:4, :].partition_broadcast(128)), writes=["lnb2"])
            def c_loads(t):
                i = t % 3
                c1, c2 = 2 * t, 2 * t + 1
                dma("pool", lambda e, i=i, c1=c1: e.indirect_dma_start(out=y1[i][:, :], out_offset=None, in_=Ys[:, :],
                                                                      in_offset=bass.IndirectOffsetOnAxis(ap=dest_i[:, c1:c1 + 1], axis=0)),
                    reads=["Ys", "dest_i"], writes=["y1_%d" % i])
                dma("pool", lambda e, i=i, c2=c2: e.indirect_dma_start(out=y2[i][:, :], out_offset=None, in_=Ys[:, :],
                                                                      in_offset=bass.IndirectOffsetOnAxis(ap=dest_i[:, c2:c2 + 1], axis=0)),
                    reads=["Ys", "dest_i"], writes=["y2_%d" % i])
                dma("pool", lambda e, i=i, t=t: e.dma_start(out=xr[i][:], in_=X1s[t * 128:(t + 1) * 128, :]), reads=["X1s"], writes=["xr_%d" % i])

            def c_front(t):
                i = t % 3
                c1, c2 = 2 * t, 2 * t + 1
                ak = "xr_%d" % i
                acc = xr[i]
                op("act", lambda e: e.activation(out=acc[:], in_=acc[:], func=AF.Copy, scale=ALPHA), reads=[ak], writes=[ak])
                op("dve", lambda e: e.scalar_tensor_tensor(out=acc[:], in0=y1[i][:], scalar=w12[:, c1:c1 + 1], in1=acc[:], op0=ALU.mult, op1=ALU.add),
                   reads=["y1_%d" % i, "w12", ak], writes=[ak])
                op("dve", lambda e: e.scalar_tensor_tensor(out=acc[:], in0=y2[i][:], scalar=w12[:, c2:c2 + 1], in1=acc[:], op0=ALU.mult, op1=ALU.add),
                   reads=["y2_%d" % i, "w12", ak], writes=[ak])
                for c in range(4):
                    op("dve", lambda e, c=c: e.bn_stats(out=stats3[i][:, c, :], in_=acc[:, c * 512:(c + 1) * 512]), reads=[ak], writes=["stats3_%d" % i])

            def c_back(t):
                i = t % 3
                ak = "xr_%d" % i
                acc = xr[i]
                layer_norm_tail(P, ak, acc, stats3[i], sm, lng, lnb, "lng2", "lnb2", stats_key="stats3_%d" % i)
                dma("sp", lambda e: e.dma_start(out=out[t * 128:(t + 1) * 128, :], in_=acc[:]), reads=[ak], writes=["out"])

            c_loads(0)
            c_loads(1)
            c_front(0)
            for t in range(16):
                if t + 2 < 16:
                    c_loads(t + 2)
                if t + 1 < 16:
                    c_front(t + 1)
                c_back(t)
            if DEBUG:
                dma("sp", lambda e: e.dma_start(out=dbg_rt[0:128, 0:4], in_=w12[:, 0:4]), reads=["w12"], writes=["dbg_rt"])
            P.finish("sp")
            P.finish("pool")
        print("n_inst", P.n_inst, "sems", 4 + len(P.dsem))
    return nc


def layer_norm_tail(P, zk, Zt, stats, sm, lng, lnb, gk, bk_, stats_key="stats"):
    op = P.op
    op("dve", lambda e: e.bn_aggr(out=sm[:, 16:18], in_=stats[:].rearrange("p a b -> p (a b)")), reads=[stats_key], writes=["sm_mv"])
    op("dve", lambda e: e.tensor_scalar(out=sm[:, 18:19], in0=sm[:, 17:18], scalar1=EPS, scalar2=None, op0=ALU.add), reads=["sm_mv"], writes=["sm_r"])
    op("act", lambda e: e.activation(out=sm[:, 18:19], in_=sm[:, 18:19], func=AF.Sqrt), reads=["sm_r"], writes=["sm_r"])
    op("dve", lambda e: e.reciprocal(out=sm[:, 18:19], in_=sm[:, 18:19]), reads=["sm_r"], writes=["sm_r"])
    op("dve", lambda e: e.scalar_tensor_tensor(out=sm[:, 19:20], in0=sm[:, 16:17], scalar=-1.0, in1=sm[:, 18:19], op0=ALU.mult, op1=ALU.mult),
       reads=["sm_mv", "sm_r"], writes=["sm_n"])
    op("act", lambda e: e.activation(out=Zt[:], in_=Zt[:], func=AF.Identity, scale=sm[:, 18:19], bias=sm[:, 19:20]),
       reads=[zk, "sm_r", "sm_n"], writes=[zk])
    op("dve", lambda e: e.tensor_tensor(out=Zt[:], in0=Zt[:], in1=lng[:], op=ALU.mult), reads=[zk, gk], writes=[zk])
    op("dve", lambda e: e.tensor_tensor(out=Zt[:], in0=Zt[:], in1=lnb[:], op=ALU.add), reads=[zk, bk_], writes=[zk])


def route_tile(P, t, pb, PB, sm, sm32, b_rb, rcb, sbase, ltri, ones_f, dest_i, w12, pre_pos_hook=None, mid_hook=None):
    op = P.op
    L = sm32[:, 0, :]
    fm = sm32[:, 1, 0:32]
    eq1 = sm32[:, 2, 0:32]
    eq2 = sm32[:, 3, 0:32]
    oh = sm32[:, 4, 0:32]
    sl = sm32[:, 5, 0:32]
    tmp = sm32[:, 6, 0:32]
    top8 = sm32[:, 7, 0:8]
    K = "rt"
    op("dve", lambda e: e.tensor_tensor(out=L, in0=pb[0][:, 0:36], in1=b_rb[:], op=ALU.add), reads=[PB[0], "b_rb"], writes=[K])
    gmax, ngmax, gsum, gp = sm[:, 20:21], sm[:, 21:22], sm[:, 22:23], sm[:, 23:24]
    op("dve", lambda e: e.reduce_max(out=gmax, in_=L[:, 0:4], axis=AX.X), reads=[K], writes=["rt_a"])
    op("dve", lambda e: e.tensor_scalar(out=ngmax, in0=gmax, scalar1=-1.0, scalar2=None, op0=ALU.mult), reads=["rt_a"], writes=["rt_b"])
    op("act", lambda e: e.activation(out=sm[:, 24:28], in_=L[:, 0:4], func=AF.Exp, bias=ngmax, accum_out=gsum), reads=[K, "rt_b"], writes=["rt_c"])
    op("dve", lambda e: e.reciprocal(out=gp, in_=gsum), reads=["rt_c"], writes=["rt_d"])
    pen = sm[:, 28:32]
    op("dve", lambda e: e.tensor_scalar(out=pen, in0=L[:, 0:4], scalar1=gmax, scalar2=None, op0=ALU.is_equal), reads=[K, "rt_a"], writes=["rt_e"])
    op("dve", lambda e: e.tensor_scalar(out=pen, in0=pen, scalar1=-1.0, scalar2=1e30, op0=ALU.add, op1=ALU.mult), reads=["rt_e"], writes=["rt_e"])
    for gi in range(4):
        op("dve", lambda e, gi=gi: e.tensor_scalar(out=fm[:, gi * 8:(gi + 1) * 8], in0=L[:, 4 + gi * 8:12 + gi * 8], scalar1=pen[:, gi:gi + 1], scalar2=None, op0=ALU.add),
           reads=[K, "rt_e"], writes=["rt_fm"])
    op("dve", lambda e: e.max(out=top8, in_=fm), reads=["rt_fm"], writes=["rt_t8"])
    op("dve", lambda e: e.tensor_scalar(out=eq1, in0=fm, scalar1=top8[:, 0:1], scalar2=None, op0=ALU.is_equal), reads=["rt_fm", "rt_t8"], writes=["rt_eq1"])
    op("dve", lambda e: e.tensor_scalar(out=eq2, in0=fm, scalar1=top8[:, 1:2], scalar2=None, op0=ALU.is_equal), reads=["rt_fm", "rt_t8"], writes=["rt_eq2"])
    dd, ee = sm[:, 32:33], sm[:, 33:34]
    op("dve", lambda e: e.tensor_tensor(out=dd, in0=top8[:, 1:2], in1=top8[:, 0:1], op=ALU.subtract), reads=["rt_t8"], writes=["rt_dd"])
    op("act", lambda e: e.activation(out=ee, in_=dd, func=AF.Exp), reads=["rt_dd"], writes=["rt_ee"])
    op("dve", lambda e: e.tensor_scalar(out=ee, in0=ee, scalar1=1.0, scalar2=None, op0=ALU.add), reads=["rt_ee"], writes=["rt_ee"])
    op("dve", lambda e: e.reciprocal(out=ee, in_=ee), reads=["rt_ee"], writes=["rt_ee"])
    c1, c2 = 2 * t, 2 * t + 1
    op("dve", lambda e: e.tensor_tensor(out=w12[:, c1:c1 + 1], in0=ee, in1=gp, op=ALU.mult), reads=["rt_ee", "rt_d"], writes=["w12"])
    op("dve", lambda e: e.tensor_tensor(out=w12[:, c2:c2 + 1], in0=gp, in1=w12[:, c1:c1 + 1], op=ALU.subtract), reads=["rt_d", "w12"], writes=["w12"])
    op("dve", lambda e: e.tensor_tensor(out=oh, in0=eq1, in1=eq2, op=ALU.add), reads=["rt_eq1", "rt_eq2"], writes=["rt_oh"])
    if pre_pos_hook is not None:
        pre_pos_hook()
    op("pe", lambda e: e.matmul(pb[1][:, 0:32], ltri[:], oh, start=True, stop=True), reads=["ltri", "rt_oh"], writes=[PB[1]])
    op("pe", lambda e: e.matmul(pb[1][:, 64:96], ones_f[:], oh, start=True, stop=True, skip_group_check=True), reads=["ones_f", "rt_oh"], writes=[PB[1]])
    if mid_hook is not None:
        mid_hook()
    DUMMY = float(NE * CAP)
    op("dve", lambda e: e.tensor_tensor(out=tmp, in0=pb[1][:, 0:32], in1=rcb[:], op=ALU.add), reads=[PB[1], "rcb"], writes=["rt_tmp"])
    op("dve", lambda e: e.tensor_tensor(out=sl, in0=tmp, in1=sbase[:], op=ALU.add), reads=["rt_tmp", "sbase"], writes=["rt_sl"])
    op("dve", lambda e: e.tensor_scalar(out=tmp, in0=tmp, scalar1=float(CAP), scalar2=None, op0=ALU.is_lt), reads=["rt_tmp"], writes=["rt_tmp"])
    op("dve", lambda e: e.scalar_tensor_tensor(out=sl, in0=sl, scalar=-DUMMY, in1=tmp, op0=ALU.add, op1=ALU.mult), reads=["rt_sl", "rt_tmp"], writes=["rt_sl"])
    op("dve", lambda e: e.tensor_scalar(out=sl, in0=sl, scalar1=DUMMY, scalar2=None, op0=ALU.add), reads=["rt_sl"], writes=["rt_sl"])
    op("dve", lambda e: e.tensor_tensor(out=rcb[:], in0=rcb[:], in1=pb[1][:, 64:96], op=ALU.add), reads=[PB[1], "rcb"], writes=["rcb"])
    d1, d2 = sm[:, 34:35], sm[:, 35:36]
    op("dve", lambda e: e.tensor_tensor(out=tmp, in0=eq1, in1=sl, op=ALU.mult), reads=["rt_eq1", "rt_sl"], writes=["rt_tmp"])
    op("dve", lambda e: e.reduce_sum(out=d1, in_=tmp, axis=AX.X), reads=["rt_tmp"], writes=["rt_d1"])
    op("dve", lambda e: e.tensor_tensor(out=tmp, in0=eq2, in1=sl, op=ALU.mult), reads=["rt_eq2", "rt_sl", "rt_d1"], writes=["rt_tmp"])
    op("dve", lambda e: e.reduce_sum(out=d2, in_=tmp, axis=AX.X), reads=["rt_tmp"], writes=["rt_d2"])
    op("dve", lambda e: e.tensor_copy(out=dest_i[:, c1:c1 + 1], in_=d1), reads=["rt_d1"], writes=["dest_i"])
    op("dve", lambda e: e.tensor_copy(out=dest_i[:, c2:c2 + 1], in_=d2), reads=["rt_d2"], writes=["dest_i"])


_NC = None


def _prep_weights(inp):
    f = np.float32
    w_in = inp["w_in"][0]
    w = {}
    w["w_in_t"] = np.ascontiguousarray(w_in.reshape(16, 128, 24, 128).transpose(2, 1, 0, 3)).reshape(24, 128, 2048)
    w["w_out_t"] = np.ascontiguousarray(inp["w_out"][0].reshape(16, 128, D).transpose(1, 0, 2))
    w["w_kv_t"] = np.ascontiguousarray(inp["w_mem_kv"][0].reshape(16, 128, 1024).transpose(1, 0, 2))
    w["w_pool_t"] = np.ascontiguousarray(inp["w_pool"][0].transpose(1, 0, 2)).reshape(128, 512)
    w["w_a_t"] = np.ascontiguousarray(inp["w_a"][0].transpose(1, 0, 2)).reshape(128, 1024)
    w["w_x_t"] = np.ascontiguousarray(inp["w_x"][0].transpose(1, 0, 2)).reshape(128, 1024)
    w["lnp"] = np.stack([inp["ln1_g"][0], inp["ln1_b"][0], inp["ln2_g"][0], inp["ln2_b"][0]]).astype(f)
    wr = np.concatenate([inp["w_group"][0], inp["w_fine"][0]], axis=1)
    w["w_r_t"] = np.ascontiguousarray(wr.reshape(16, 128, 36).transpose(1, 0, 2)).reshape(128, 16 * 36)
    w["b_r"] = np.concatenate([inp["b_group"][0], inp["b_fine"][0].reshape(-1)]).reshape(1, 36).astype(f)
    w["wg_t"] = np.ascontiguousarray(inp["w_gate"][0].reshape(NE, 16, 128, 2, 512).transpose(0, 3, 2, 1, 4)).reshape(NE, 2, 128, 16 * 512)
    w["wu_t"] = np.ascontiguousarray(inp["w_up"][0].reshape(NE, 16, 128, 2, 512).transpose(0, 3, 2, 1, 4)).reshape(NE, 2, 128, 16 * 512)
    w["wd_t"] = np.ascontiguousarray(inp["w_down"][0].reshape(NE, 8, 128, 2, 1024).transpose(0, 3, 2, 1, 4)).reshape(NE, 2, 128, 8 * 1024)
    chv = np.zeros((128, NV), f)
    chv[:, O_PS:O_PS + 4] = inp["pool_scale"][0].reshape(4, 128).T
    cw = inp["conv_w"][0]
    chv[:, O_CW:O_CW + 32] = cw.reshape(4, 8, 128).transpose(2, 1, 0).reshape(128, 32)
    chv[:, O_CB:O_CB + 8] = inp["conv_b"][0].reshape(8, 128).T
    chv[:, O_BA:O_BA + 8] = inp["b_a"][0].reshape(8, 128).T
    chv[:, O_BX:O_BX + 8] = inp["b_x"][0].reshape(8, 128).T
    chv[:, O_LAM:O_LAM + 8] = inp["lam"][0].reshape(8, 128).T
    chv[:, O_G:O_G + 16] = inp["mix_norm_g"][0].reshape(16, 128).T
    return w, chv


def kernel(**inp):
    global _NC
    inp = {k: np.asarray(v) for k, v in inp.items()}
    x = inp["x"]
    mem = inp["mem"]
    w, chv0 = _prep_weights(inp)
    in_maps = []
    for c in range(8):
        b, q = c // 4, c % 4
        n = (q + 1) * T
        xe = np.zeros((NCH * CH, D), np.float32)
        xe[NCH * CH - n:] = x[b, :n]
        chv = chv0.copy()
        vm = np.zeros(16, np.float32)
        vm[16 - 4 * (q + 1):] = 1.0
        chv[:, O_VM:O_VM + 16] = vm[None, :]
        invc = np.zeros((128, 64), np.float32)
        for gi in range(4):
            wl = 2 ** (gi + 1)
            for tt in range(16):
                invc[:, gi * 16 + tt] = 1.0 / (min(tt + 1, wl) if q == 0 else wl)
        m = dict(w)
        m.update({"xe": xe, "mem": np.ascontiguousarray(mem[b]), "chv": chv, "invcnt": invc})
        in_maps.append(m)
    if _NC is None:
        _NC = build_nc()
    res = run_bass_kernel_spmd(_NC, in_maps, core_ids=list(range(8)))
    outs = [res.results[c]["out"] for c in range(8)]
    full = np.stack(outs).reshape(2, 4 * T, D).astype(np.float32)
    kernel.last_results = res.results
    return full
```

```python
import numpy as np
from contextlib import ExitStack
import concourse.bass as bass
import concourse.mybir as mybir
from concourse.bass_utils import run_bass_kernel_spmd

F32 = mybir.dt.float32
F32R = mybir.dt.float32r
BF16 = mybir.dt.bfloat16
I32 = mybir.dt.int32
AF = mybir.ActivationFunctionType
ALU = mybir.AluOpType
AX = mybir.AxisListType

D = 2048
T = 2048
NCH = 16
NPRE = 12
CH = 512
CAP = 384
NSB = CAP // 128
NE = 32
ALPHA = 2.0 ** 0.25
EPS = 1e-5
DEBUG = False

O_PS, O_CW, O_CB, O_BA, O_BX, O_LAM, O_G, O_VM = 0, 4, 36, 44, 52, 60, 68, 84
NV = 100


class Prog:
    def __init__(self, nc, stack):
        self.nc = nc
        self.stack = stack
        self.eng = {"pe": nc.tensor, "act": nc.scalar, "dve": nc.vector,
                    "pool": nc.gpsimd, "sp": nc.sync}
        self.sem = {}
        for e in ("pe", "act", "dve", "pool"):
            self.sem[e] = stack.enter_context(nc.semaphore("sem_" + e))
        self.cnt = {e: 0 for e in ("pe", "act", "dve", "pool")}
        self.dsem = {}
        self.dcnt = {}
        self.last_w = {}
        self.readers = {}
        self.waited = {e: {} for e in ("pe", "act", "dve", "pool", "sp")}
        self.n_inst = 0

    def _tok_sem(self, tok):
        return self.sem[tok[1]] if tok[0] == "e" else self.dsem[tok[1]]

    def _wait(self, eng, tok):
        kind, key, val = tok
        name = (kind, key)
        if self.waited[eng].get(name, 0) >= val:
            return
        if kind == "e" and key == eng and eng == "pe":
            return
        self.waited[eng][name] = val
        self.eng[eng].wait_ge(self._tok_sem(tok), val)

    def _deps(self, eng, reads, writes):
        deps = []
        for r in reads:
            t = self.last_w.get(r)
            if t is not None:
                deps.append(t)
        for w in writes:
            t = self.last_w.get(w)
            if t is not None:
                deps.append(t)
            for t in self.readers.get(w, ()):
                deps.append(t)
        return deps

    def _commit(self, tok, reads, writes):
        for r in reads:
            self.readers.setdefault(r, []).append(tok)
        for w in writes:
            self.last_w[w] = tok
            self.readers[w] = []

    def op(self, eng, fn, reads=(), writes=(), inc=True):
        for t in self._deps(eng, reads, writes):
            self._wait(eng, t)
        ins = fn(self.eng[eng])
        self.n_inst += 1
        if inc:
            self.cnt[eng] += 1
            ins.then_inc(self.sem[eng], 1)
            tok = ("e", eng, self.cnt[eng])
        else:
            tok = ("e", eng, self.cnt[eng] + 1)
        self._commit(tok, reads, writes)
        return ins

    def dma(self, q, fn, reads=(), writes=(), key=None):
        if key is None:
            key = writes[0]
        if key not in self.dsem:
            self.dsem[key] = self.stack.enter_context(self.nc.semaphore("d_" + str(key)))
            self.dcnt[key] = 0
        for t in self._deps(q, reads, writes):
            self._wait(q, t)
        ins = fn(self.eng[q])
        self.n_inst += 1
        self.dcnt[key] += 16
        ins.then_inc(self.dsem[key], 16)
        tok = ("d", key, self.dcnt[key])
        self._commit(tok, reads, writes)
        return ins

    def _all_tokens(self):
        toks = [("e", e, self.cnt[e]) for e in ("pe", "act", "dve", "pool") if self.cnt[e] > 0]
        toks += [("d", k, v) for k, v in self.dcnt.items() if v > 0]
        return toks

    def barrier(self):
        toks = self._all_tokens()
        for e in ("pe", "act", "dve", "pool", "sp"):
            for t in toks:
                self._wait(e, t)
        self.last_w.clear()
        self.readers.clear()

    def finish(self, eng):
        for t in self._all_tokens():
            self._wait(eng, t)


def build_nc():
    nc = bass.Bass("TRN2", target_bir_lowering=False)
    nc.dge_precook = False
    dram = lambda name, shape, dt, kind="ExternalInput": nc.dram_tensor(name, shape, dt, kind=kind)
    xe = dram("xe", [NCH * CH, D], F32)
    mem = dram("mem", [256, D], F32)
    w_in_t = dram("w_in_t", [24, 128, 16 * 128], F32)
    w_out_t = dram("w_out_t", [128, 16, D], F32)
    w_kv_t = dram("w_kv_t", [128, 16, 1024], F32)
    w_pool_t = dram("w_pool_t", [128, 4 * 128], F32)
    w_a_t = dram("w_a_t", [128, 8 * 128], F32)
    w_x_t = dram("w_x_t", [128, 8 * 128], F32)
    chv_d = dram("chv", [128, NV], F32)
    invc_d = dram("invcnt", [128, 64], F32)
    lnp = dram("lnp", [4, D], F32)
    w_r_t = dram("w_r_t", [128, 16 * 36], F32)
    b_r = dram("b_r", [1, 36], F32)
    wg_t = dram("wg_t", [NE, 2, 128, 16 * 512], F32R)
    wu_t = dram("wu_t", [NE, 2, 128, 16 * 512], F32R)
    wd_t = dram("wd_t", [NE, 2, 128, 8 * 1024], F32R)
    out = dram("out", [T, D], F32, kind="ExternalOutput")
    Xs = dram("Xs", [NE * CAP, D], F32, kind="Internal")
    Ys = dram("Ys", [NE * CAP + 1, D], F32, kind="Internal")
    X1s = dram("X1s", [T, D], F32, kind="Internal")
    if DEBUG:
        dbg_x1 = dram("dbg_x1", [T, D], F32, kind="ExternalOutput")
        dbg_rt = dram("dbg_rt", [T, 4], F32, kind="ExternalOutput")

    with ExitStack() as g:
        P = Prog(nc, g)

        def sbin(st, name, shape, dt):
            return st.enter_context(nc.sbuf_tensor(name, shape, dt))

        pb = [g.enter_context(nc.psum_tensor("pb%d" % i, [128, 512], F32)) for i in range(8)]
        PB = ["pb%d" % i for i in range(8)]

        ident = sbin(g, "ident", [128, 128], F32)
        ones_f = sbin(g, "ones_f", [128, 128], F32)
        ones_b = sbin(g, "ones_b", [128, 128], BF16)
        ltri = sbin(g, "ltri", [128, 128], F32)
        chv = sbin(g, "chv_s", [128, NV], F32)
        invc = sbin(g, "invc_s", [128, 64], F32)
        cl = sbin(g, "cl", [128, 32], F32)
        vmh = sbin(g, "vmh", [128, 16], F32)
        wpool_b = sbin(g, "wpool_b", [128, 4 * 128], BF16)
        wa_b = sbin(g, "wa_b", [128, 8 * 128], BF16)
        wx_b = sbin(g, "wx_b", [128, 8 * 128], BF16)
        kT = sbin(g, "kT", [128, 4, 256], BF16)
        vv = sbin(g, "vv", [128, 2, 512], BF16)
        hst = sbin(g, "hst", [128, 8], F32)
        w_r = sbin(g, "w_r", [128, 16, 36], F32)
        b_rb = sbin(g, "b_rb", [128, 36], F32)
        rcb = sbin(g, "rcb", [128, 32], F32)
        sbase = sbin(g, "sbase", [128, 32], F32)
        dest_i = sbin(g, "dest_i", [128, 32], I32)
        w12 = sbin(g, "w12", [128, 32], F32)
        sm = sbin(g, "sm", [128, 64], F32)
        sm32 = sbin(g, "sm32", [128, 8, 36], F32)

        dma = P.dma
        op = P.op

        op("pool", lambda e: e.memset(ident[:], 0.0), writes=["ident"])
        op("pool", lambda e: e.affine_select(out=ident[:], in_=ident[:], pattern=[[-1, 128]],
                                             compare_op=ALU.not_equal, fill=1.0, base=0, channel_multiplier=1),
           reads=["ident"], writes=["ident"])
        op("pool", lambda e: e.memset(ones_f[:], 1.0), writes=["ones_f"])
        op("pool", lambda e: e.memset(ones_b[:], 1.0), writes=["ones_b"])
        op("pool", lambda e: e.affine_select(out=ltri[:], in_=ones_f[:], pattern=[[1, 128]],
                                             compare_op=ALU.is_gt, fill=0.0, base=0, channel_multiplier=-1),
           reads=["ones_f"], writes=["ltri"])
        op("pool", lambda e: e.memset(hst[:], 0.0), writes=["hst"])
        op("pool", lambda e: e.iota(dest_i[:], pattern=[[CAP, 32]], base=0, channel_multiplier=0), writes=["dest_i"])
        op("dve", lambda e: e.tensor_copy(out=sbase[:], in_=dest_i[:]), reads=["dest_i"], writes=["sbase"])
        op("pool", lambda e: e.memset(rcb[:], 0.0), writes=["rcb"])
        dma("sp", lambda e: e.dma_start(out=chv[:], in_=chv_d.ap()), writes=["chv"])
        dma("sp", lambda e: e.dma_start(out=invc[:], in_=invc_d.ap()), writes=["invc"])
        dma("sp", lambda e: e.dma_start(out=w_r[:], in_=w_r_t.ap().rearrange("p (k n) -> p k n", n=36)), writes=["w_r"])
        dma("sp", lambda e: e.dma_start(out=b_rb[:], in_=b_r.ap().partition_broadcast(128)), writes=["b_rb"])
        op("act", lambda e: e.activation(out=sm[:, 0:8], in_=chv[:, O_LAM:O_LAM + 8], func=AF.Exp, scale=-1.0),
           reads=["chv"], writes=["sm"])
        op("act", lambda e: e.activation(out=sm[:, 8:16], in_=sm[:, 0:8], func=AF.Ln, bias=1.0),
           reads=["sm"], writes=["sm"])
        op("dve", lambda e: e.tensor_scalar_mul(out=cl[:, 0:8], in0=sm[:, 8:16], scalar1=-4.0), reads=["sm"], writes=["cl"])
        op("dve", lambda e: e.tensor_scalar_mul(out=cl[:, 8:16], in0=sm[:, 8:16], scalar1=-8.0), reads=["sm"], writes=["cl"])
        op("dve", lambda e: e.tensor_scalar_mul(out=cl[:, 16:24], in0=chv[:, O_BA:O_BA + 8], scalar1=0.5), reads=["chv"], writes=["cl"])
        op("dve", lambda e: e.tensor_scalar_mul(out=cl[:, 24:32], in0=chv[:, O_BX:O_BX + 8], scalar1=0.5), reads=["chv"], writes=["cl"])
        op("dve", lambda e: e.tensor_scalar_mul(out=vmh[:], in0=chv[:, O_VM:O_VM + 16], scalar1=0.5), reads=["chv"], writes=["vmh"])

        with ExitStack() as sa:
            stg = sbin(sa, "stgA", [128, 1024], F32)
            memx = sbin(sa, "memx", [128, 2, D], F32)
            memT = sbin(sa, "memT", [128, 16, 256], BF16)
            wkv_s = [sbin(sa, "wkv_s%d" % i, [128, 1024], F32) for i in range(4)]
            wkv_b = [sbin(sa, "wkv_b%d" % i, [128, 1024], BF16) for i in range(4)]
            for (src, dstb, n, nm) in ((w_pool_t, wpool_b, 512, "wpool_b"), (w_a_t, wa_b, 1024, "wa_b"), (w_x_t, wx_b, 1024, "wx_b")):
                dma("sp", lambda e, src=src, n=n: e.dma_start(out=stg[:, 0:n], in_=src.ap()), writes=["stgA"])
                op("dve", lambda e, dstb=dstb, n=n: e.tensor_copy(out=dstb[:], in_=stg[:, 0:n]), reads=["stgA"], writes=[nm])
            for mt in range(2):
                dma("sp", lambda e, mt=mt: e.dma_start(out=memx[:, mt, :], in_=mem[mt * 128:(mt + 1) * 128, :]), writes=["memx%d" % mt])
            for mt in range(2):
                for g4 in range(4):
                    bk = g4 % 2
                    for i4 in range(4):
                        kc = g4 * 4 + i4
                        op("pe", lambda e, mt=mt, kc=kc, i4=i4, bk=bk: e.transpose(pb[bk][:, i4 * 128:(i4 + 1) * 128], memx[:, mt, kc * 128:(kc + 1) * 128], ident[:]),
                           reads=["memx%d" % mt, "ident"], writes=[PB[bk]], inc=(i4 == 3))
                    op("act" if g4 % 2 == 0 else "dve",
                       (lambda e, mt=mt, g4=g4, bk=bk: e.activation(out=memT[:, g4 * 4:(g4 + 1) * 4, mt * 128:(mt + 1) * 128], in_=pb[bk][:].rearrange("p (a b) -> p a b", b=128), func=AF.Copy))
                       if g4 % 2 == 0 else
                       (lambda e, mt=mt, g4=g4, bk=bk: e.tensor_copy(out=memT[:, g4 * 4:(g4 + 1) * 4, mt * 128:(mt + 1) * 128], in_=pb[bk][:].rearrange("p (a b) -> p a b", b=128))),
                       reads=[PB[bk]], writes=["memT"])
            for kc in range(16):
                s = kc % 4
                dma("sp", lambda e, kc=kc, s=s: e.dma_start(out=wkv_s[s][:], in_=w_kv_t[:, kc, :]), writes=["wkv_s%d" % s])
                op("dve", lambda e, s=s: e.tensor_copy(out=wkv_b[s][:], in_=wkv_s[s][:]), reads=["wkv_s%d" % s], writes=["wkv_b%d" % s])
                for h in range(4):
                    bk = h
                    op("pe", lambda e, h=h, kc=kc, s=s, bk=bk: e.matmul(pb[bk][:, 0:256], wkv_b[s][:, h * 128:(h + 1) * 128], memT[:, kc, :],
                                                                        start=(kc == 0), stop=(kc == 15)),
                       reads=["wkv_b%d" % s, "memT"], writes=[PB[bk]], inc=False)
                for mc in range(2):
                    op("pe", lambda e, mc=mc, kc=kc, s=s: e.matmul(pb[4 + mc][:], memT[:, kc, mc * 128:(mc + 1) * 128], wkv_b[s][:, 512:1024],
                                                                   start=(kc == 0), stop=(kc == 15)),
                       reads=["wkv_b%d" % s, "memT"], writes=[PB[4 + mc]], inc=(mc == 1))
            for h in range(4):
                bk = h
                op("act", lambda e, h=h, bk=bk: e.activation(out=kT[:, h, :], in_=pb[bk][:, 0:256], func=AF.Copy),
                   reads=[PB[bk]], writes=["kT"])
            for mc in range(2):
                op("dve", lambda e, mc=mc: e.tensor_copy(out=vv[:, mc, :], in_=pb[4 + mc][:]), reads=[PB[4 + mc]], writes=["vv"])
            P.barrier()

        with ExitStack() as sbs:
            yT = sbin(sbs, "yT", [128, 16, T], BF16)
            sp1 = sbs.enter_context(ExitStack())
            uext = sbin(sp1, "uext", [128, 8, CH + 3], BF16)
            upx = sbin(sp1, "upx", [128, 4, CH + 16], F32)
            op("pool", lambda e: e.memset(uext[:], 0.0), writes=["uext%d" % i for i in range(8)])
            op("pool", lambda e: e.memset(upx[:], 0.0), writes=["upx%d" % i for i in range(4)])
            yT_box = [yT]
            dg = sbin(sp1, "dg", [128, 8, 4, 128], BF16)
            for blk_ in range(8):
                for k_ in range(4):
                    cwc = O_CW + blk_ * 4 + k_
                    op("dve" if (blk_ * 4 + k_) % 2 == 0 else "pool",
                       lambda e, blk_=blk_, k_=k_, cwc=cwc: e.tensor_scalar(out=dg[:, blk_, k_, :], in0=ident[:], scalar1=chv[:, cwc:cwc + 1], scalar2=None, op0=ALU.mult),
                       reads=["ident", "chv"], writes=["dg"])

            def phase1(chunks, NS, own_phase):
              with ExitStack() as s1:
                yT = yT_box[0]
                tag = "o_" if own_phase else "p_"
                xin = [sbin(s1, tag + "xin%d" % i, [128, D], F32) for i in range(2)]
                xT = [sbin(s1, tag + "xT%d" % i, [128, 16, CH], BF16) for i in range(2)]
                wbf = [sbin(s1, tag + "wbf%d" % i, [128, 16, 128], BF16) for i in range(3)]
                NW = 9
                A = []
                for si in range(2):
                    tl = [sbin(s1, tag + "A%d_%d" % (si, i), [128, CH], F32) for i in range(4)]
                    A.append([tl[0][:], tl[1][:], tl[2][:], tl[3][:], tl[1][:]])
                ucbs = sbin(s1, tag + "ucbs", [128, 2, CH], BF16)
                ucb_aps = [ucbs[:, 0, :], ucbs[:, 1, :]]
                if NS > 2:
                    def ht(i):
                        return yT[:, i // 2, :].bitcast(F32)[:, (i % 2) * CH:(i % 2 + 1) * CH]
                    for si in range(2, NS):
                        tl = [ht(4 * (si - 2) + j_) for j_ in range(4)]
                        A.append([tl[0], tl[1], tl[2], tl[3], tl[1]])
                        ucb_aps.append(yT[:, 12 + (si - 2) // 4, ((si - 2) % 4) * CH:((si - 2) % 4 + 1) * CH])

                def akeys(st):
                    return ["A%d_0" % st, "A%d_1" % st, "A%d_2" % st, "A%d_3" % st, "A%d_1" % st]

                if own_phase:
                    W = [None] * 6 + [sbin(s1, tag + "W%d" % i, [128, CH + 16], F32) for i in range(6, NW)]
                    Wb = [None, sbin(s1, tag + "Wb1", [128, 2, CH], BF16), sbin(s1, tag + "Wb2", [128, 1, CH], BF16)]
                    vg = sbin(s1, tag + "vg", [128, 3, CH], F32)

                items = []
                for ci in chunks:
                    own = ci >= NPRE
                    for pr in range(4):
                        for blk in (2 * pr, 2 * pr + 1):
                            items.append((ci, 4 + blk, "u", blk))
                        if own:
                            items.append((ci, pr, "p", pr))
                            for blk in (2 * pr, 2 * pr + 1):
                                items.append((ci, 12 + blk, "g", blk))
                            items.append((ci, 20 + pr, "m", pr))
                    if ci == NPRE - 1:
                        for gi in range(4):
                            items.append((ci, gi, "ph", gi))
                NI = len(items)

                def load_w(n):
                    ci, j, kind, idx = items[n]
                    s = n % 3
                    dma("pool", lambda e: e.dma_start(out=wbf[s][:].rearrange("p a b -> p (a b)"), in_=w_in_t[j], max_dma_last_dim=4096),
                        writes=["wbf%d" % s])

                def prep_chunk(ci):
                    b = ci % 2
                    for tt in range(4):
                        xi = tt % 2
                        r0 = ci * CH + tt * 128
                        dma("sp", lambda e: e.dma_start(out=xin[xi][:], in_=xe[r0:r0 + 128, :]), writes=["xin%d" % xi])
                        for g4 in range(4):
                            bk = 0
                            for i4 in range(4):
                                kc = g4 * 4 + i4
                                op("pe", lambda e, kc=kc, i4=i4: e.transpose(pb[bk][:, i4 * 128:(i4 + 1) * 128], xin[xi][:, kc * 128:(kc + 1) * 128], ident[:]),
                                   reads=["xin%d" % xi, "ident"], writes=[PB[bk]], inc=(i4 == 3))
                            dst = xT[b][:, g4 * 4:(g4 + 1) * 4, tt * 128:(tt + 1) * 128]
                            src = pb[bk][:].rearrange("p (a b) -> p a b", b=128)
                            if g4 % 2 == 0:
                                op("act", lambda e: e.activation(out=dst, in_=src, func=AF.Copy), reads=[PB[bk]], writes=["xT%d" % b])
                            else:
                                op("dve", lambda e: e.tensor_copy(out=dst, in_=src), reads=[PB[bk]], writes=["xT%d" % b])

                def inproj(n, bank, ncols=CH, c0=0):
                    ci, j, kind, idx = items[n]
                    s = n % 3
                    b = ci % 2
                    for kc in range(16):
                        op("pe", lambda e, kc=kc: e.matmul(pb[bank][:, 0:ncols], wbf[s][:, kc, :], xT[b][:, kc, c0:c0 + ncols],
                                                          start=(kc == 0), stop=(kc == 15)),
                           reads=["wbf%d" % s, "xT%d" % b], writes=[PB[bank]], inc=(kc == 15))

                def ssq_accum(ytile_key, ysrc, first, last, gidx=1):
                    if gidx == 1:
                        op("act", lambda e: e.activation(out=Wb[2][:, 0, :], in_=ysrc, func=AF.Square), reads=[ytile_key], writes=["Wb2_0"])
                        op("pe", lambda e: e.matmul(pb[6][:], ones_b[:], Wb[2][:, 0, :], start=first, stop=last),
                           reads=["Wb2_0", "ones_b"], writes=[PB[6]], inc=True)
                        return
                    vk = "vg%d" % gidx
                    if first:
                        op("act", lambda e: e.activation(out=vg[:, gidx, :], in_=ysrc, func=AF.Square), reads=[ytile_key], writes=[vk])
                    else:
                        op("act", lambda e: e.activation(out=W[6][:, 0:CH], in_=ysrc, func=AF.Square), reads=[ytile_key], writes=["W6"])
                        op("pool", lambda e: e.tensor_tensor(out=vg[:, gidx, :], in0=vg[:, gidx, :], in1=W[6][:, 0:CH], op=ALU.add),
                           reads=[vk, "W6"], writes=[vk])
                    if last:
                        op("pe", lambda e: e.matmul(pb[4][:], ones_f[:], vg[:, gidx, :], start=True, stop=True),
                           reads=["ones_f", vk], writes=[PB[4]])

                def group_var(gidx, n_ch):
                    bank = 6 if gidx == 1 else 4
                    op("dve", lambda e: e.tensor_scalar(out=vg[:, gidx, :], in0=pb[bank][:], scalar1=1.0 / n_ch, scalar2=EPS, op0=ALU.mult, op1=ALU.add),
                       reads=[PB[bank]], writes=["vg%d" % gidx])

                setof = {}

                def ubank(n):
                    return (2, 3, 6)[n % 3] if not own_phase else 2 + (n % 2)

                def lru_s1(n):
                    lru_s1a(n)
                    lru_s1b(n)

                def lru_s1a(n):
                    ci, j, kind, blk = items[n]
                    bank = ubank(n)
                    inproj(n, bank)
                    ue = "uext%d" % blk
                    op("dve", lambda e: e.tensor_copy(out=uext[:, blk, 0:3], in_=uext[:, blk, CH:CH + 3]), reads=[ue], writes=[ue])
                    op("dve", lambda e: e.tensor_copy(out=uext[:, blk, 3:CH + 3], in_=pb[bank][:]), reads=[PB[bank]], writes=[ue])

                def lru_s1b(n):
                    ci, j, kind, blk = items[n]
                    st = setof[n]
                    ucb = ucb_aps[st]
                    ucbk = "ucb%d" % st
                    bank = ubank(n)
                    ue = "uext%d" % blk
                    for k in range(4):
                        op("pe", lambda e, k=k: e.matmul(pb[bank][:], dg[:, blk, k, :], uext[:, blk, k:k + CH], start=(k == 0), stop=(k == 3)),
                           reads=["dg", ue], writes=[PB[bank]], inc=(k == 3))
                    op("act", lambda e: e.activation(out=ucb, in_=pb[bank][:], func=AF.Identity, bias=chv[:, O_CB + blk:O_CB + blk + 1]),
                       reads=[PB[bank], "chv"], writes=[ucbk])

                def lru_s2(n):
                    ci, j, kind, blk = items[n]
                    st = setof[n]
                    AK = akeys(st)
                    acc, thr, thi, aa, a2 = A[st]
                    ucb = ucb_aps[st]
                    ucbk = "ucb%d" % st
                    pr_, pi_ = (4, 5) if st % 2 == 0 else (1, 7)
                    op("pe", lambda e: e.matmul(pb[pr_][:], wa_b[:, blk * 128:(blk + 1) * 128], ucb, start=True, stop=True),
                       reads=["wa_b", ucbk], writes=[PB[pr_]])
                    op("pe", lambda e: e.matmul(pb[pi_][:], wx_b[:, blk * 128:(blk + 1) * 128], ucb, start=True, stop=True),
                       reads=["wx_b", ucbk], writes=[PB[pi_]])
                    op("act", lambda e: e.activation(out=thr[:], in_=pb[pr_][:], func=AF.Tanh, scale=0.5, bias=cl[:, 16 + blk:17 + blk]),
                       reads=[PB[pr_], "cl"], writes=[AK[1]])
                    op("act", lambda e: e.activation(out=thi[:], in_=pb[pi_][:], func=AF.Tanh, scale=0.5, bias=cl[:, 24 + blk:25 + blk]),
                       reads=[PB[pi_], "cl"], writes=[AK[2]])
                    op("act", lambda e: e.activation(out=aa[:], in_=thr[:], func=AF.Exp, scale=cl[:, blk:blk + 1], bias=cl[:, blk:blk + 1]),
                       reads=[AK[1], "cl"], writes=[AK[3]])
                    op("act", lambda e: e.activation(out=a2[:], in_=thr[:], func=AF.Exp, scale=cl[:, 8 + blk:9 + blk], bias=cl[:, 8 + blk:9 + blk]),
                       reads=[AK[1], "cl"], writes=[AK[4]])

                def lru_s3(n):
                    ci, j, kind, blk = items[n]
                    st = setof[n]
                    AK = akeys(st)
                    acc, thr, thi, aa, a2 = A[st]
                    ucb = ucb_aps[st]
                    ucbk = "ucb%d" % st
                    op("dve", lambda e: e.tensor_scalar_min(out=a2[:], in0=a2[:], scalar1=0.9999999), reads=[AK[4]], writes=[AK[4]])
                    op("dve", lambda e: e.scalar_tensor_tensor(out=thi[:], in0=thi[:], scalar=1.0, in1=ucb, op0=ALU.add, op1=ALU.mult),
                       reads=[AK[2], ucbk], writes=[AK[2]])

                def lru_s4a(n):
                    ci, j, kind, blk = items[n]
                    st = setof[n]
                    AK = akeys(st)
                    acc, thr, thi, aa, a2 = A[st]
                    op("act", lambda e: e.activation(out=a2[:], in_=a2[:], func=AF.Sqrt, scale=-1.0, bias=1.0), reads=[AK[4]], writes=[AK[4]])

                def lru_s4b(n):
                    ci, j, kind, blk = items[n]
                    st = setof[n]
                    AK = akeys(st)
                    acc, thr, thi, aa, a2 = A[st]
                    op("dve", lambda e: e.scalar_tensor_tensor(out=a2[:], in0=a2[:], scalar=vmh[:, ci:ci + 1], in1=thi[:], op0=ALU.mult, op1=ALU.mult),
                       reads=[AK[4], AK[2], "vmh"], writes=[AK[4]])
                    op("dve", lambda e: e.tensor_tensor_scan(out=acc[:], data0=aa[:], data1=a2[:], initial=hst[:, blk:blk + 1],
                                                             op0=ALU.mult, op1=ALU.add),
                       reads=[AK[3], AK[4], "hst"], writes=[AK[0]])
                    op("pool", lambda e: e.tensor_copy(out=hst[:, blk:blk + 1], in_=acc[:, CH - 1:CH]), reads=[AK[0]], writes=["hst"])

                def lru_g(n, do_inproj=True):
                    ci, j, kind, blk = items[n]
                    st = blk % 2
                    hk = "A%d_0" % st
                    oc = ci - NPRE
                    bank = 2 + (n % 2)
                    if do_inproj:
                        inproj(n, bank)
                    gg = W[6]
                    op("act", lambda e: e.activation(out=gg[:, 0:CH], in_=pb[bank][:], func=AF.Gelu_apprx_tanh), reads=[PB[bank]], writes=["W6"])
                    ysl = yT[:, 4 + blk, oc * CH:(oc + 1) * CH]
                    yk = "yT%d" % (4 + blk)
                    op("dve", lambda e: e.tensor_tensor(out=ysl, in0=gg[:, 0:CH], in1=A[st][0][:], op=ALU.mult), reads=["W6", hk], writes=[yk])
                    ssq_accum(yk, ysl, blk == 0, blk == 7)
                    if blk == 7:
                        group_var(1, 1024.0)

                def pool_blk(n, halo_only=False, do_inproj=True):
                    ci, j, kind, gi = items[n]
                    bank = 2 + (n % 2)
                    pk = "upx%d" % gi
                    if halo_only:
                        inproj(n, bank, ncols=16, c0=CH - 16)
                        op("act", lambda e: e.activation(out=upx[:, gi, CH:CH + 16], in_=pb[bank][:, 0:16], func=AF.Copy), reads=[PB[bank]], writes=[pk])
                        return
                    oc = ci - NPRE
                    if do_inproj:
                        inproj(n, bank)
                    op("dve", lambda e: e.tensor_copy(out=upx[:, gi, 0:16], in_=upx[:, gi, CH:CH + 16]), reads=[pk], writes=[pk])
                    op("act", lambda e: e.activation(out=upx[:, gi, 16:CH + 16], in_=pb[bank][:], func=AF.Copy), reads=[PB[bank]], writes=[pk])
                    src = upx[:, gi, :]
                    srck = pk
                    step = 1
                    lo = 0
                    for lvl in range(gi + 1):
                        dstt = W[7 + (lvl % 2)]
                        dk = "W%d" % (7 + (lvl % 2))
                        lo2 = lo + step
                        op("pool", lambda e, src=src, dstt=dstt, lo2=lo2, step=step: e.tensor_tensor(out=dstt[:, lo2:CH + 16], in0=src[:, lo2:CH + 16], in1=src[:, lo2 - step:CH + 16 - step], op=ALU.add),
                           reads=[srck], writes=[dk])
                        src, srck, lo, step = dstt, dk, lo2, step * 2
                    wlen = float(2 ** (gi + 1))
                    dbf = Wb[1][:, 0, :]
                    op("dve", lambda e: e.scalar_tensor_tensor(out=dbf, in0=src[:, 16:CH + 16], scalar=1.0 / wlen, in1=upx[:, gi, 16:CH + 16], op0=ALU.mult, op1=ALU.subtract),
                       reads=[srck, pk], writes=["Wb1_0"])
                    if oc == 0:
                        tmp = W[6]
                        op("dve", lambda e: e.tensor_tensor(out=tmp[:, 0:16], in0=src[:, 16:32], in1=invc[:, gi * 16:(gi + 1) * 16], op=ALU.mult),
                           reads=[srck, "invc"], writes=["W6"])
                        op("dve", lambda e: e.tensor_tensor(out=Wb[1][:, 0, 0:16], in0=tmp[:, 0:16], in1=upx[:, gi, 16:32], op=ALU.subtract),
                           reads=["W6", pk, "Wb1_0"], writes=["Wb1_0"])
                    op("pe", lambda e: e.matmul(pb[4][:], wpool_b[:, gi * 128:(gi + 1) * 128], dbf, start=True, stop=True),
                       reads=["wpool_b", "Wb1_0"], writes=[PB[4]])
                    ysl = yT[:, gi, oc * CH:(oc + 1) * CH]
                    yk = "yT%d" % gi
                    op("act", lambda e: e.activation(out=ysl, in_=pb[4][:], func=AF.Identity, scale=chv[:, O_PS + gi:O_PS + gi + 1]),
                       reads=[PB[4], "chv"], writes=[yk])
                    ssq_accum(yk, ysl, gi == 0, gi == 3, gidx=0)
                    if gi == 3:
                        group_var(0, 512.0)

                def mem_blk(n, do_inproj=True):
                    ci, j, kind, h = items[n]
                    oc = ci - NPRE
                    bank = 2 + (n % 2)
                    if do_inproj:
                        inproj(n, bank)
                    qT = Wb[1][:, 1, :]
                    op("act", lambda e: e.activation(out=qT, in_=pb[bank][:], func=AF.Copy, scale=128.0 ** -0.5), reads=[PB[bank]], writes=["Wb1_1"])
                    for mc in range(2):
                        op("pe", lambda e, mc=mc: e.matmul(pb[4 + mc][:], kT[:, h, mc * 128:(mc + 1) * 128], qT, start=True, stop=True),
                           reads=["kT", "Wb1_1"], writes=[PB[4 + mc]])
                    for mc in range(2):
                        op("act", lambda e, mc=mc: e.activation(out=ucbs[:, mc, :], in_=pb[4 + mc][:], func=AF.Exp), reads=[PB[4 + mc]], writes=["ucb%d" % mc])
                    for mc in range(2):
                        op("pe", lambda e, mc=mc: e.matmul(pb[4][:], vv[:, mc, h * 128:(h + 1) * 128], ucbs[:, mc, :], start=(mc == 0), stop=(mc == 1)),
                           reads=["vv", "ucb%d" % mc], writes=[PB[4]], inc=(mc == 1))
                    for mc in range(2):
                        op("pe", lambda e, mc=mc: e.matmul(pb[5][:], ones_b[:], ucbs[:, mc, :], start=(mc == 0), stop=(mc == 1)),
                           reads=["ones_b", "ucb%d" % mc], writes=[PB[5]], inc=(mc == 1))
                    rinv = W[6]
                    op("dve", lambda e: e.reciprocal(out=rinv[:, 0:CH], in_=pb[5][:]), reads=[PB[5]], writes=["W6"])
                    ysl = yT[:, 12 + h, oc * CH:(oc + 1) * CH]
                    yk = "yT%d" % (12 + h)
                    op("dve", lambda e: e.tensor_tensor(out=ysl, in0=pb[4][:], in1=rinv[:, 0:CH], op=ALU.mult), reads=[PB[4], "W6"], writes=[yk])
                    ssq_accum(yk, ysl, h == 0, h == 3, gidx=2)
                    if h == 3:
                        group_var(2, 512.0)

                def finish_chunk(ci):
                    oc = ci - NPRE
                    for gidx in range(3):
                        op("act", lambda e, gidx=gidx: e.activation(out=vg[:, gidx, :], in_=vg[:, gidx, :], func=AF.Sqrt), reads=["vg%d" % gidx], writes=["vg%d" % gidx])
                    for gidx in range(3):
                        op("dve", lambda e, gidx=gidx: e.reciprocal(out=vg[:, gidx, :], in_=vg[:, gidx, :]), reads=["vg%d" % gidx], writes=["vg%d" % gidx])
                    for blk16 in range(16):
                        gidx = 0 if blk16 < 4 else (1 if blk16 < 12 else 2)
                        ysl = yT[:, blk16, oc * CH:(oc + 1) * CH]
                        eng = "dve" if blk16 % 2 == 0 else "pool"
                        op(eng, lambda e, ysl=ysl, gidx=gidx: e.tensor_tensor(out=ysl, in0=ysl, in1=vg[:, gidx, :], op=ALU.mult),
                           reads=["yT%d" % blk16, "vg%d" % gidx], writes=["yT%d" % blk16])

                def prefetch(n):
                    if n + 2 < NI:
                        load_w(n + 2)

                prep_chunk(chunks[0])
                load_w(0)
                if NI > 1:
                    load_w(1)
                if not own_phase:
                    GS = NS // 2
                    us = [n for n in range(NI) if items[n][2] == "u"]
                    rest = [n for n in range(NI) if items[n][2] != "u"]
                    upairs = [tuple(us[i:i + GS]) for i in range(0, len(us), GS)]
                    for g_, grp in enumerate(upairs):
                        for ii, n in enumerate(grp):
                            setof[n] = (g_ % 2) * GS + ii
                    G = len(upairs)
                    for g_ in range(G + 1):
                        prev = upairs[g_ - 1] if g_ >= 1 else None
                        cur = upairs[g_] if g_ < G else None
                        for i in range(GS + 1):
                            if cur and i < GS:
                                prefetch(cur[i])
                                lru_s1a(cur[i])
                                ci, j, kind, idx = items[cur[i]]
                                if i == 0 and idx == 0 and ci + 1 <= chunks[-1]:
                                    prep_chunk(ci + 1)
                            if cur and i >= 1:
                                lru_s1b(cur[i - 1])
                            if prev and i >= 1:
                                lru_s2(prev[i - 1])
                                lru_s3(prev[i - 1])
                        if prev:
                            for n in prev:
                                lru_s4a(n)
                            for n in prev:
                                lru_s4b(n)
                    for n in rest:
                        prefetch(n)
                        pool_blk(n, halo_only=True)
                else:
                    def ip(m):
                        prefetch(m)
                        inproj(m, 2 + (m % 2))

                    n = 0
                    while n < NI:
                        ci, j, kind, idx = items[n]
                        na, nb, np_, nga, ngb, nm = n, n + 1, n + 2, n + 3, n + 4, n + 5
                        setof[na], setof[nb] = 0, 1
                        for m in (na, nb):
                            prefetch(m)
                            lru_s1a(m)
                        if idx == 0 and ci + 1 <= chunks[-1]:
                            prep_chunk(ci + 1)
                        for m in (na, nb):
                            lru_s1b(m)
                        ip(np_)
                        for m in (na, nb):
                            lru_s2(m)
                        ip(nga)
                        pool_blk(np_, do_inproj=False)
                        for m in (na, nb):
                            lru_s3(m)
                        for m in (na, nb):
                            lru_s4a(m)
                        for m in (na, nb):
                            lru_s4b(m)
                        lru_g(nga, do_inproj=False)
                        ip(ngb)
                        ip(nm)
                        lru_g(ngb, do_inproj=False)
                        mem_blk(nm, do_inproj=False)
                        last_of_chunk = (nm + 1 == NI) or (items[nm + 1][0] != ci)
                        if last_of_chunk:
                            finish_chunk(ci)
                        n += 6
                P.barrier()

            phase1(list(range(0, NPRE)), 8, False)
            phase1(list(range(NPRE, NCH)), 2, True)
            sp1.close()

            with ExitStack() as s2:
                woutg = sbin(s2, "woutg", [128, 16, D], BF16)
                with ExitStack() as s2a:
                    wos = [sbin(s2a, "wos%d" % i, [128, D], F32) for i in range(6)]
                    for kb in range(16):
                        s = kb % 6
                        dma("sp", lambda e, kb=kb, s=s: e.dma_start(out=wos[s][:], in_=w_out_t[:, kb, :]), writes=["wos%d" % s])
                        op("dve",
                           lambda e, kb=kb, s=s: e.tensor_scalar(out=woutg[:, kb, :], in0=wos[s][:], scalar1=chv[:, O_G + kb:O_G + kb + 1], scalar2=None, op0=ALU.mult),
                           reads=["wos%d" % s, "chv"], writes=["woutg"])
                    P.barrier()
                xin2 = [sbin(s2, "xin2_%d" % i, [128, D], F32) for i in range(2)]
                Z = [sbin(s2, "Z%d" % i, [128, D], F32) for i in range(2)]
                x1T = sbin(s2, "x1T", [128, 16, 128], F32)
                lng = sbin(s2, "lng", [128, D], F32)
                lnb = sbin(s2, "lnb", [128, D], F32)
                stats2 = [sbin(s2, "stats_%d" % i, [128, 4, 6], F32) for i in range(2)]
                dma("sp", lambda e: e.dma_start(out=lng[:], in_=lnp[0:1, :].partition_broadcast(128)), writes=["lng"])
                dma("sp", lambda e: e.dma_start(out=lnb[:], in_=lnp[1:2, :].partition_broadcast(128)), writes=["lnb"])
                def p2_load(t):
                    zi = t % 2
                    r0 = NPRE * CH + t * 128
                    dma("sp", lambda e: e.dma_start(out=xin2[zi][:], in_=xe[r0:r0 + 128, :]), writes=["xin2_%d" % zi])

                def p2_mm(t, cs):
                    for c in cs:
                        bank = 2 + (c % 2)
                        for kb in range(16):
                            op("pe", lambda e, kb=kb, c=c, bank=bank: e.matmul(pb[bank][:], yT[:, kb, t * 128:(t + 1) * 128], woutg[:, kb, c * 512:(c + 1) * 512],
                                                                              start=(kb == 0), stop=(kb == 15)),
                               reads=["yT", "woutg"], writes=[PB[bank]], inc=(kb == 15))

                def p2_evac(t, cs):
                    zi = t % 2
                    zk = "Z%d" % zi
                    xk = "xin2_%d" % zi
                    for c in cs:
                        bank = 2 + (c % 2)
                        op("dve", lambda e, c=c, bank=bank: e.scalar_tensor_tensor(out=Z[zi][:, c * 512:(c + 1) * 512], in0=xin2[zi][:, c * 512:(c + 1) * 512], scalar=ALPHA,
                                                                                   in1=pb[bank][:], op0=ALU.mult, op1=ALU.add),
                           reads=[xk, PB[bank]], writes=[zk])
                        op("dve", lambda e, c=c: e.bn_stats(out=stats2[zi][:, c, :], in_=Z[zi][:, c * 512:(c + 1) * 512]), reads=[zk], writes=["stats_%d" % zi])

                def p2_back(t):
                    zi = t % 2
                    zk = "Z%d" % zi
                    nxt = t + 1 if t + 1 < 16 else None
                    layer_norm_tail(P, zk, Z[zi], stats2[zi], sm, lng, lnb, "lng", "lnb", stats_key="stats_%d" % zi)
                    if nxt is not None:
                        p2_load(nxt)
                    for g4 in range(4):
                        bk = 4 + g4
                        for i4 in range(4):
                            kc = g4 * 4 + i4
                            op("pe", lambda e, kc=kc, i4=i4, bk=bk: e.transpose(pb[bk][:, i4 * 128:(i4 + 1) * 128], Z[zi][:, kc * 128:(kc + 1) * 128], ident[:]),
                               reads=[zk, "ident"], writes=[PB[bk]], inc=(i4 == 3))
                        op("act", lambda e, g4=g4, bk=bk: e.activation(out=x1T[:, g4 * 4:(g4 + 1) * 4, :], in_=pb[bk][:].rearrange("p (a b) -> p a b", b=128), func=AF.Copy),
                           reads=[PB[bk]], writes=["x1T"])
                    for kc in range(16):
                        op("pe", lambda e, kc=kc: e.matmul(pb[0][:, 0:36], x1T[:, kc, :], w_r[:, kc, :], start=(kc == 0), stop=(kc == 15)),
                           reads=["x1T", "w_r"], writes=[PB[0]], inc=(kc == 15))
                    route_tile(P, t, pb, PB, sm, sm32, b_rb, rcb, sbase, ltri, ones_f, dest_i, w12,
                               pre_pos_hook=(lambda: p2_mm(nxt, (0, 1))) if nxt is not None else None,
                               mid_hook=(lambda: p2_evac(nxt, (0, 1))) if nxt is not None else None)
                    if nxt is not None:
                        p2_mm(nxt, (2, 3))
                        p2_evac(nxt, (2, 3))
                    for k in range(2):
                        col = 2 * t + k
                        dma("pool", lambda e, col=col: e.indirect_dma_start(out=Xs[:, :], out_offset=bass.IndirectOffsetOnAxis(ap=dest_i[:, col:col + 1], axis=0),
                                                                            in_=Z[zi][:, :], in_offset=None, bounds_check=NE * CAP - 1, oob_is_err=False),
                            reads=[zk, "dest_i"], writes=["Xs"])
                    dma("pool", lambda e: e.dma_start(out=X1s[t * 128:(t + 1) * 128, :], in_=Z[zi][:]), reads=[zk], writes=["X1s"])
                    if DEBUG:
                        dma("sp", lambda e: e.dma_start(out=dbg_x1[t * 128:(t + 1) * 128, :], in_=Z[zi][:]), reads=[zk], writes=["dbg_x1"])
                p2_load(0)
                p2_mm(0, (0, 1))
                p2_evac(0, (0, 1))
                p2_mm(0, (2, 3))
                p2_evac(0, (2, 3))
                for t in range(16):
                    p2_back(t)
                P.barrier()

        with ExitStack() as s3:
            ring = [sbin(s3, "ring%d" % i, [128, 16 * 512], F32R) for i in range(3)]
            xs = [sbin(s3, "xs%d" % i, [128, D], F32) for i in range(2)]
            XT = [sbin(s3, "XT%d" % i, [128, 16, CAP], F32R) for i in range(2)]
            hT = sbin(s3, "hT", [128, 8, CAP], F32R)
            sg = [sbin(s3, "sg%d" % i, [128, CAP], F32) for i in range(2)]
            yo = [sbin(s3, "yo%d" % i, [128, 1024], F32) for i in range(3)]
            op("pool", lambda e: e.memset(sg[0][0:1, :], 0.0), writes=["sg0"])
            for cz in range(D // CAP + 1):
                w_ = min(CAP, D - cz * CAP)
                if w_ <= 0:
                    break
                dma("pool", lambda e, cz=cz, w_=w_: e.dma_start(out=Ys[NE * CAP:NE * CAP + 1, cz * CAP:cz * CAP + w_], in_=sg[0][0:1, 0:w_]),
                    reads=["sg0"], writes=["Ys"])
            pieces = []
            for e_ in range(NE):
                for n_ in range(2):
                    pieces.append((e_, "g", n_))
                    pieces.append((e_, "u", n_))
                for h_ in range(2):
                    pieces.append((e_, "d", h_))
            NP_ = len(pieces)
            yo_ctr = [0]

            def load_piece(i):
                e_, kind, n_ = pieces[i]
                r = i % 3
                src = {"g": wg_t, "u": wu_t, "d": wd_t}[kind]
                dma("sp", lambda e: e.dma_start(out=ring[r][:], in_=src[e_, n_]), writes=["ring%d" % r])

            def prep_expert(e_):
                b = e_ % 2
                for s in range(NSB):
                    xi = s % 2
                    r0 = e_ * CAP + s * 128
                    dma("sp", lambda e, xi=xi, r0=r0: e.dma_start(out=xs[xi][:], in_=Xs[r0:r0 + 128, :]), reads=["Xs"], writes=["xs%d" % xi])
                    for g4 in range(4):
                        bk = g4 % 2
                        for i4 in range(4):
                            kc = g4 * 4 + i4
                            op("pe", lambda e, xi=xi, kc=kc, i4=i4, bk=bk: e.transpose(pb[bk][:, i4 * 128:(i4 + 1) * 128], xs[xi][:, kc * 128:(kc + 1) * 128], ident[:]),
                               reads=["xs%d" % xi, "ident"], writes=[PB[bk]], inc=(i4 == 3))
                        op("act" if g4 % 2 == 0 else "dve",
                           (lambda e, s=s, g4=g4, bk=bk: e.activation(out=XT[b][:, g4 * 4:(g4 + 1) * 4, s * 128:(s + 1) * 128], in_=pb[bk][:].rearrange("p (a b) -> p a b", b=128), func=AF.Copy))
                           if g4 % 2 == 0 else
                           (lambda e, s=s, g4=g4, bk=bk: e.tensor_copy(out=XT[b][:, g4 * 4:(g4 + 1) * 4, s * 128:(s + 1) * 128], in_=pb[bk][:].rearrange("p (a b) -> p a b", b=128))),
                           reads=[PB[bk]], writes=["XT%d" % b])

            load_piece(0)
            load_piece(1)
            prep_expert(0)
            for i in range(NP_):
                e_, kind, n_ = pieces[i]
                b = e_ % 2
                r = i % 3
                if kind == "g":
                    if i + 2 < NP_:
                        load_piece(i + 2)
                    continue
                if kind == "u":
                    rg = (i - 1) % 3
                    for f in range(4):
                        fc = n_ * 4 + f
                        bg = 2 + (fc % 2)
                        bu = 4 + (fc % 2)
                        for kc in range(16):
                            op("pe", lambda e, kc=kc, f=f, bg=bg, rg=rg: e.matmul(pb[bg][:, 0:CAP], ring[rg][:, kc * 512 + f * 128:kc * 512 + (f + 1) * 128], XT[b][:, kc, :],
                                                                                  start=(kc == 0), stop=(kc == 15)),
                               reads=["ring%d" % rg, "XT%d" % b], writes=[PB[bg]], inc=(kc == 15))
                        for kc in range(16):
                            op("pe", lambda e, kc=kc, f=f, bu=bu: e.matmul(pb[bu][:, 0:CAP], ring[r][:, kc * 512 + f * 128:kc * 512 + (f + 1) * 128], XT[b][:, kc, :],
                                                                          start=(kc == 0), stop=(kc == 15)),
                               reads=["ring%d" % r, "XT%d" % b], writes=[PB[bu]], inc=(kc == 15))
                        si = fc % 2
                        op("act", lambda e, bg=bg, si=si: e.activation(out=sg[si][:], in_=pb[bg][:, 0:CAP], func=AF.Silu), reads=[PB[bg]], writes=["sg%d" % si])
                        op("dve", lambda e, bu=bu, si=si, fc=fc: e.tensor_tensor(out=hT[:, fc, :], in0=sg[si][:], in1=pb[bu][:, 0:CAP], op=ALU.mult),
                           reads=["sg%d" % si, PB[bu]], writes=["hT"])
                    if n_ == 1 and e_ + 1 < NE:
                        prep_expert(e_ + 1)
                else:
                    for s in range(NSB):
                        yi = yo_ctr[0] % 3
                        yo_ctr[0] += 1
                        for c in range(2):
                            bank = 6 + c
                            for fc in range(8):
                                op("pe", lambda e, fc=fc, s=s, c=c, bank=bank: e.matmul(pb[bank][:], hT[:, fc, s * 128:(s + 1) * 128], ring[r][:, fc * 1024 + c * 512:fc * 1024 + (c + 1) * 512],
                                                                                      start=(fc == 0), stop=(fc == 7)),
                                   reads=["hT", "ring%d" % r], writes=[PB[bank]], inc=(fc == 7))
                            if c == 0:
                                op("act", lambda e, yi=yi, bank=bank, c=c: e.activation(out=yo[yi][:, c * 512:(c + 1) * 512], in_=pb[bank][:], func=AF.Copy),
                                   reads=[PB[bank]], writes=["yo%d" % yi])
                            else:
                                op("dve", lambda e, yi=yi, bank=bank, c=c: e.tensor_copy(out=yo[yi][:, c * 512:(c + 1) * 512], in_=pb[bank][:]),
                                   reads=[PB[bank]], writes=["yo%d" % yi])
                        r0 = e_ * CAP + s * 128
                        dma("pool", lambda e, yi=yi, r0=r0, n_=n_: e.dma_start(out=Ys[r0:r0 + 128, n_ * 1024:(n_ + 1) * 1024], in_=yo[yi][:]),
                            reads=["yo%d" % yi], writes=["Ys"])
                if i + 2 < NP_:
                    load_piece(i + 2)
            P.barrier()

        with ExitStack() as s4:
            y1 = [sbin(s4, "y1_%d" % i, [128, D], F32) for i in range(3)]
            y2 = [sbin(s4, "y2_%d" % i, [128, D], F32) for i in range(3)]
            xr = [sbin(s4, "xr_%d" % i, [128, D], F32) for i in range(3)]
            lng = sbin(s4, "lng2", [128, D], F32)
            lnb = sbin(s4, "lnb2", [128, D], F32)
            stats3 = [sbin(s4, "stats3_%d" % i, [128, 4, 6], F32) for i in range(3)]
            dma("sp", lambda e: e.dma_start(out=lng[:], in_=lnp[2:3, :].partition_broadcast(128)), writes=["lng2"])
            dma("sp", lambda e: e.dma_start(out=lnb[:], in_=lnp[3:4, :].partition_broadcast(128)), writes=["lnb2"])
            def c_loads(t):
                i = t % 3
                c1, c2 = 2 * t, 2 * t + 1
                dma("pool", lambda e, i=i, c1=c1: e.indirect_dma_start(out=y1[i][:, :], out_offset=None, in_=Ys[:, :],
                                                                      in_offset=bass.IndirectOffsetOnAxis(ap=dest_i[:, c1:c1 + 1], axis=0)),
                    reads=["Ys", "dest_i"], writes=["y1_%d" % i])
                dma("pool", lambda e, i=i, c2=c2: e.indirect_dma_start(out=y2[i][:, :], out_offset=None, in_=Ys[:, :],
                                                                      in_offset=bass.IndirectOffsetOnAxis(ap=dest_i[:, c2:c2 + 1], axis=0)),
                    reads=["Ys", "dest_i"], writes=["y2_%d" % i])
                dma("pool", lambda e, i=i, t=t: e.dma_start(out=xr[i][:], in_=X1s[t * 128:(t + 1) * 128, :]), reads=["X1s"], writes=["xr_%d" % i])

            def c_front(t):
                i = t % 3
                c1, c2 = 2 * t, 2 * t + 1
                ak = "xr_%d" % i
                acc = xr[i]
                op("act", lambda e: e.activation(out=acc[:], in_=acc[:], func=AF.Copy, scale=ALPHA), reads=[ak], writes=[ak])
                op("dve", lambda e: e.scalar_tensor_tensor(out=acc[:], in0=y1[i][:], scalar=w12[:, c1:c1 + 1], in1=acc[:], op0=ALU.mult, op1=ALU.add),
                   reads=["y1_%d" % i, "w12", ak], writes=[ak])
                op("dve", lambda e: e.scalar_tensor_tensor(out=acc[:], in0=y2[i][:], scalar=w12[:, c2:c2 + 1], in1=acc[:], op0=ALU.mult, op1=ALU.add),
                   reads=["y2_%d" % i, "w12", ak], writes=[ak])
                for c in range(4):
                    op("dve", lambda e, c=c: e.bn_stats(out=stats3[i][:, c, :], in_=acc[:, c * 512:(c + 1) * 512]), reads=[ak], writes=["stats3_%d" % i])

            def c_back(t):
                i = t % 3
                ak = "xr_%d" % i
                acc = xr[i]
                layer_norm_tail(P, ak, acc, stats3[i], sm, lng, lnb, "lng2", "lnb2", stats_key="stats3_%d" % i)
                dma("sp", lambda e: e.dma_start(out=out[t * 128:(t + 1) * 128, :], in_=acc[:]), reads=[ak], writes=["out"])

            c_loads(0)
            c_loads(1)
            c_front(0)
            for t in range(16):
                if t + 2 < 16:
                    c_loads(t + 2)
                if t + 1 < 16:
                    c_front(t + 1)
                c_back(t)
            if DEBUG:
                dma("sp", lambda e: e.dma_start(out=dbg_rt[0:128, 0:4], in_=w12[:, 0:4]), reads=["w12"], writes=["dbg_rt"])
            P.finish("sp")
            P.finish("pool")
        print("n_inst", P.n_inst, "sems", 4 + len(P.dsem))
    return nc


def layer_norm_tail(P, zk, Zt, stats, sm, lng, lnb, gk, bk_, stats_key="stats"):
    op = P.op
    op("dve", lambda e: e.bn_aggr(out=sm[:, 16:18], in_=stats[:].rearrange("p a b -> p (a b)")), reads=[stats_key], writes=["sm_mv"])
    op("dve", lambda e: e.tensor_scalar(out=sm[:, 18:19], in0=sm[:, 17:18], scalar1=EPS, scalar2=None, op0=ALU.add), reads=["sm_mv"], writes=["sm_r"])
    op("act", lambda e: e.activation(out=sm[:, 18:19], in_=sm[:, 18:19], func=AF.Sqrt), reads=["sm_r"], writes=["sm_r"])
    op("dve", lambda e: e.reciprocal(out=sm[:, 18:19], in_=sm[:, 18:19]), reads=["sm_r"], writes=["sm_r"])
    op("dve", lambda e: e.scalar_tensor_tensor(out=sm[:, 19:20], in0=sm[:, 16:17], scalar=-1.0, in1=sm[:, 18:19], op0=ALU.mult, op1=ALU.mult),
       reads=["sm_mv", "sm_r"], writes=["sm_n"])
    op("act", lambda e: e.activation(out=Zt[:], in_=Zt[:], func=AF.Identity, scale=sm[:, 18:19], bias=sm[:, 19:20]),
       reads=[zk, "sm_r", "sm_n"], writes=[zk])
    op("dve", lambda e: e.tensor_tensor(out=Zt[:], in0=Zt[:], in1=lng[:], op=ALU.mult), reads=[zk, gk], writes=[zk])
    op("dve", lambda e: e.tensor_tensor(out=Zt[:], in0=Zt[:], in1=lnb[:], op=ALU.add), reads=[zk, bk_], writes=[zk])


def route_tile(P, t, pb, PB, sm, sm32, b_rb, rcb, sbase, ltri, ones_f, dest_i, w12, pre_pos_hook=None, mid_hook=None):
    op = P.op
    L = sm32[:, 0, :]
    fm = sm32[:, 1, 0:32]
    eq1 = sm32[:, 2, 0:32]
    eq2 = sm32[:, 3, 0:32]
    oh = sm32[:, 4, 0:32]
    sl = sm32[:, 5, 0:32]
    tmp = sm32[:, 6, 0:32]
    top8 = sm32[:, 7, 0:8]
    K = "rt"
    op("dve", lambda e: e.tensor_tensor(out=L, in0=pb[0][:, 0:36], in1=b_rb[:], op=ALU.add), reads=[PB[0], "b_rb"], writes=[K])
    gmax, ngmax, gsum, gp = sm[:, 20:21], sm[:, 21:22], sm[:, 22:23], sm[:, 23:24]
    op("dve", lambda e: e.reduce_max(out=gmax, in_=L[:, 0:4], axis=AX.X), reads=[K], writes=["rt_a"])
    op("dve", lambda e: e.tensor_scalar(out=ngmax, in0=gmax, scalar1=-1.0, scalar2=None, op0=ALU.mult), reads=["rt_a"], writes=["rt_b"])
    op("act", lambda e: e.activation(out=sm[:, 24:28], in_=L[:, 0:4], func=AF.Exp, bias=ngmax, accum_out=gsum), reads=[K, "rt_b"], writes=["rt_c"])
    op("dve", lambda e: e.reciprocal(out=gp, in_=gsum), reads=["rt_c"], writes=["rt_d"])
    pen = sm[:, 28:32]
    op("dve", lambda e: e.tensor_scalar(out=pen, in0=L[:, 0:4], scalar1=gmax, scalar2=None, op0=ALU.is_equal), reads=[K, "rt_a"], writes=["rt_e"])
    op("dve", lambda e: e.tensor_scalar(out=pen, in0=pen, scalar1=-1.0, scalar2=1e30, op0=ALU.add, op1=ALU.mult), reads=["rt_e"], writes=["rt_e"])
    for gi in range(4):
        op("dve", lambda e, gi=gi: e.tensor_scalar(out=fm[:, gi * 8:(gi + 1) * 8], in0=L[:, 4 + gi * 8:12 + gi * 8], scalar1=pen[:, gi:gi + 1], scalar2=None, op0=ALU.add),
           reads=[K, "rt_e"], writes=["rt_fm"])
    op("dve", lambda e: e.max(out=top8, in_=fm), reads=["rt_fm"], writes=["rt_t8"])
    op("dve", lambda e: e.tensor_scalar(out=eq1, in0=fm, scalar1=top8[:, 0:1], scalar2=None, op0=ALU.is_equal), reads=["rt_fm", "rt_t8"], writes=["rt_eq1"])
    op("dve", lambda e: e.tensor_scalar(out=eq2, in0=fm, scalar1=top8[:, 1:2], scalar2=None, op0=ALU.is_equal), reads=["rt_fm", "rt_t8"], writes=["rt_eq2"])
    dd, ee = sm[:, 32:33], sm[:, 33:34]
    op("dve", lambda e: e.tensor_tensor(out=dd, in0=top8[:, 1:2], in1=top8[:, 0:1], op=ALU.subtract), reads=["rt_t8"], writes=["rt_dd"])
    op("act", lambda e: e.activation(out=ee, in_=dd, func=AF.Exp), reads=["rt_dd"], writes=["rt_ee"])
    op("dve", lambda e: e.tensor_scalar(out=ee, in0=ee, scalar1=1.0, scalar2=None, op0=ALU.add), reads=["rt_ee"], writes=["rt_ee"])
    op("dve", lambda e: e.reciprocal(out=ee, in_=ee), reads=["rt_ee"], writes=["rt_ee"])
    c1, c2 = 2 * t, 2 * t + 1
    op("dve", lambda e: e.tensor_tensor(out=w12[:, c1:c1 + 1], in0=ee, in1=gp, op=ALU.mult), reads=["rt_ee", "rt_d"], writes=["w12"])
    op("dve", lambda e: e.tensor_tensor(out=w12[:, c2:c2 + 1], in0=gp, in1=w12[:, c1:c1 + 1], op=ALU.subtract), reads=["rt_d", "w12"], writes=["w12"])
    op("dve", lambda e: e.tensor_tensor(out=oh, in0=eq1, in1=eq2, op=ALU.add), reads=["rt_eq1", "rt_eq2"], writes=["rt_oh"])
    if pre_pos_hook is not None:
        pre_pos_hook()
    op("pe", lambda e: e.matmul(pb[1][:, 0:32], ltri[:], oh, start=True, stop=True), reads=["ltri", "rt_oh"], writes=[PB[1]])
    op("pe", lambda e: e.matmul(pb[1][:, 64:96], ones_f[:], oh, start=True, stop=True, skip_group_check=True), reads=["ones_f", "rt_oh"], writes=[PB[1]])
    if mid_hook is not None:
        mid_hook()
    DUMMY = float(NE * CAP)
    op("dve", lambda e: e.tensor_tensor(out=tmp, in0=pb[1][:, 0:32], in1=rcb[:], op=ALU.add), reads=[PB[1], "rcb"], writes=["rt_tmp"])
    op("dve", lambda e: e.tensor_tensor(out=sl, in0=tmp, in1=sbase[:], op=ALU.add), reads=["rt_tmp", "sbase"], writes=["rt_sl"])
    op("dve", lambda e: e.tensor_scalar(out=tmp, in0=tmp, scalar1=float(CAP), scalar2=None, op0=ALU.is_lt), reads=["rt_tmp"], writes=["rt_tmp"])
    op("dve", lambda e: e.scalar_tensor_tensor(out=sl, in0=sl, scalar=-DUMMY, in1=tmp, op0=ALU.add, op1=ALU.mult), reads=["rt_sl", "rt_tmp"], writes=["rt_sl"])
    op("dve", lambda e: e.tensor_scalar(out=sl, in0=sl, scalar1=DUMMY, scalar2=None, op0=ALU.add), reads=["rt_sl"], writes=["rt_sl"])
    op("dve", lambda e: e.tensor_tensor(out=rcb[:], in0=rcb[:], in1=pb[1][:, 64:96], op=ALU.add), reads=[PB[1], "rcb"], writes=["rcb"])
    d1, d2 = sm[:, 34:35], sm[:, 35:36]
    op("dve", lambda e: e.tensor_tensor(out=tmp, in0=eq1, in1=sl, op=ALU.mult), reads=["rt_eq1", "rt_sl"], writes=["rt_tmp"])
    op("dve", lambda e: e.reduce_sum(out=d1, in_=tmp, axis=AX.X), reads=["rt_tmp"], writes=["rt_d1"])
    op("dve", lambda e: e.tensor_tensor(out=tmp, in0=eq2, in1=sl, op=ALU.mult), reads=["rt_eq2", "rt_sl", "rt_d1"], writes=["rt_tmp"])
    op("dve", lambda e: e.reduce_sum(out=d2, in_=tmp, axis=AX.X), reads=["rt_tmp"], writes=["rt_d2"])
    op("dve", lambda e: e.tensor_copy(out=dest_i[:, c1:c1 + 1], in_=d1), reads=["rt_d1"], writes=["dest_i"])
    op("dve", lambda e: e.tensor_copy(out=dest_i[:, c2:c2 + 1], in_=d2), reads=["rt_d2"], writes=["dest_i"])


_NC = None


def _prep_weights(inp):
    f = np.float32
    w_in = inp["w_in"][0]
    w = {}
    w["w_in_t"] = np.ascontiguousarray(w_in.reshape(16, 128, 24, 128).transpose(2, 1, 0, 3)).reshape(24, 128, 2048)
    w["w_out_t"] = np.ascontiguousarray(inp["w_out"][0].reshape(16, 128, D).transpose(1, 0, 2))
    w["w_kv_t"] = np.ascontiguousarray(inp["w_mem_kv"][0].reshape(16, 128, 1024).transpose(1, 0, 2))
    w["w_pool_t"] = np.ascontiguousarray(inp["w_pool"][0].transpose(1, 0, 2)).reshape(128, 512)
    w["w_a_t"] = np.ascontiguousarray(inp["w_a"][0].transpose(1, 0, 2)).reshape(128, 1024)
    w["w_x_t"] = np.ascontiguousarray(inp["w_x"][0].transpose(1, 0, 2)).reshape(128, 1024)
    w["lnp"] = np.stack([inp["ln1_g"][0], inp["ln1_b"][0], inp["ln2_g"][0], inp["ln2_b"][0]]).astype(f)
    wr = np.concatenate([inp["w_group"][0], inp["w_fine"][0]], axis=1)
    w["w_r_t"] = np.ascontiguousarray(wr.reshape(16, 128, 36).transpose(1, 0, 2)).reshape(128, 16 * 36)
    w["b_r"] = np.concatenate([inp["b_group"][0], inp["b_fine"][0].reshape(-1)]).reshape(1, 36).astype(f)
    w["wg_t"] = np.ascontiguousarray(inp["w_gate"][0].reshape(NE, 16, 128, 2, 512).transpose(0, 3, 2, 1, 4)).reshape(NE, 2, 128, 16 * 512)
    w["wu_t"] = np.ascontiguousarray(inp["w_up"][0].reshape(NE, 16, 128, 2, 512).transpose(0, 3, 2, 1, 4)).reshape(NE, 2, 128, 16 * 512)
    w["wd_t"] = np.ascontiguousarray(inp["w_down"][0].reshape(NE, 8, 128, 2, 1024).transpose(0, 3, 2, 1, 4)).reshape(NE, 2, 128, 8 * 1024)
    chv = np.zeros((128, NV), f)
    chv[:, O_PS:O_PS + 4] = inp["pool_scale"][0].reshape(4, 128).T
    cw = inp["conv_w"][0]
    chv[:, O_CW:O_CW + 32] = cw.reshape(4, 8, 128).transpose(2, 1, 0).reshape(128, 32)
    chv[:, O_CB:O_CB + 8] = inp["conv_b"][0].reshape(8, 128).T
    chv[:, O_BA:O_BA + 8] = inp["b_a"][0].reshape(8, 128).T
    chv[:, O_BX:O_BX + 8] = inp["b_x"][0].reshape(8, 128).T
    chv[:, O_LAM:O_LAM + 8] = inp["lam"][0].reshape(8, 128).T
    chv[:, O_G:O_G + 16] = inp["mix_norm_g"][0].reshape(16, 128).T
    return w, chv


def kernel(**inp):
    global _NC
    inp = {k: np.asarray(v) for k, v in inp.items()}
    x = inp["x"]
    mem = inp["mem"]
    w, chv0 = _prep_weights(inp)
    in_maps = []
    for c in range(8):
        b, q = c // 4, c % 4
        n = (q + 1) * T
        xe = np.zeros((NCH * CH, D), np.float32)
        xe[NCH * CH - n:] = x[b, :n]
        chv = chv0.copy()
        vm = np.zeros(16, np.float32)
        vm[16 - 4 * (q + 1):] = 1.0
        chv[:, O_VM:O_VM + 16] = vm[None, :]
        invc = np.zeros((128, 64), np.float32)
        for gi in range(4):
            wl = 2 ** (gi + 1)
            for tt in range(16):
                invc[:, gi * 16 + tt] = 1.0 / (min(tt + 1, wl) if q == 0 else wl)
        m = dict(w)
        m.update({"xe": xe, "mem": np.ascontiguousarray(mem[b]), "chv": chv, "invcnt": invc})
        in_maps.append(m)
    if _NC is None:
        _NC = build_nc()
    res = run_bass_kernel_spmd(_NC, in_maps, core_ids=list(range(8)))
    outs = [res.results[c]["out"] for c in range(8)]
    full = np.stack(outs).reshape(2, 4 * T, D).astype(np.float32)
    kernel.last_results = res.results
    return full
```
